# Optimizing a Trainium2 kernel written in Bass

```python
import numpy as np
import jax, jax.numpy as jnp
from jax import lax

D_MODEL = 1024
BATCH = 32
SEQ = 2048
DEPTH = 2

HEAD_DIM = 64
MOBA_HEADS = D_MODEL // (2 * HEAD_DIM)
NSA_HEADS = D_MODEL // (2 * HEAD_DIM)
NSA_KV_GROUPS = 2
NSA_HPG = NSA_HEADS // NSA_KV_GROUPS
MOBA_BLOCK = 256
MOBA_TOPK = 3
CMP_LEN = 32
CMP_STRIDE = 16
SLC_BLOCK = 64
SLC_TOPN = 16
WINDOW = 512
Q_CHUNK = 128
RMS_EPS = 1e-6
NEG = -1e9
FORCE_BONUS = 1e4

MOBA_W = MOBA_HEADS * HEAD_DIM
NSA_W = NSA_HEADS * HEAD_DIM
KV_W = NSA_KV_GROUPS * HEAD_DIM
SPLITS = [MOBA_W] * 4 + [NSA_W] + [KV_W] * 6 + [3 * NSA_HEADS] + [NSA_W]
D_IN = sum(SPLITS)

kernel_name = "hybrid_moba_nsa_sandwich_adaln"


def rmsnorm(x, g):
    xf = x.astype(jnp.float32)
    y = xf * lax.rsqrt(jnp.mean(xf * xf, axis=-1, keepdims=True) + RMS_EPS)
    return (y * g.astype(jnp.float32)).astype(x.dtype)


def alibi_slopes(n):
    return jnp.asarray(2.0 ** (-8.0 * np.arange(1, n + 1) / n), dtype=jnp.float32)


def cmp_to_slc_matrix(n_cmp, n_slc):
    i = np.arange(n_cmp)[:, None]
    j = np.arange(n_slc)[None, :]
    start = i * CMP_STRIDE
    end = start + CMP_LEN
    ov = (start < (j + 1) * SLC_BLOCK) & (end > j * SLC_BLOCK)
    return jnp.asarray(ov.astype(np.float32))


def compress_blocks(k, pe, w1, w2):
    B, S, G, dh = k.shape
    T = (S - CMP_LEN) // CMP_STRIDE + 1
    idx = np.arange(T)[:, None] * CMP_STRIDE + np.arange(CMP_LEN)[None, :]
    win = k[:, idx] + pe[:, None, :]
    win = win.transpose(0, 3, 1, 2, 4).reshape(B, G, T, CMP_LEN * dh)
    return jax.nn.gelu(win @ w1) @ w2


def hybrid_mixer(h, w_in, w_out, pe_k, pe_v, w_ck1, w_ck2, w_cv1, w_cv2):
    B, S, _ = h.shape
    dh = HEAD_DIM
    NC = S // Q_CHUNK
    NB = -(-S // MOBA_BLOCK)
    KM = min(MOBA_TOPK, NB)
    NBS = S // SLC_BLOCK
    NS = min(SLC_TOPN, NBS)
    T = (S - CMP_LEN) // CMP_STRIDE + 1
    scale = dh ** -0.5

    u = h @ w_in
    offs = np.cumsum(SPLITS)[:-1].tolist()
    (qm, km, vm, zm, qn, kc, vc, ks, vs, kw, vw, gl, zn) = jnp.split(u, offs, axis=-1)

    def heads(a, n):
        return a.reshape(B, S, n, dh).transpose(0, 2, 1, 3)

    pad = NB * MOBA_BLOCK - S
    qm = heads(qm, MOBA_HEADS) * scale
    km = jnp.pad(heads(km, MOBA_HEADS), ((0, 0), (0, 0), (0, pad), (0, 0))).reshape(B, MOBA_HEADS, NB, MOBA_BLOCK, dh)
    vm = jnp.pad(heads(vm, MOBA_HEADS), ((0, 0), (0, 0), (0, pad), (0, 0))).reshape(B, MOBA_HEADS, NB, MOBA_BLOCK, dh)
    kmean = jnp.mean(km, axis=3)

    G = NSA_KV_GROUPS
    qn = qn.reshape(B, S, G, NSA_HPG, dh).transpose(0, 2, 3, 1, 4) * scale
    kc = compress_blocks(kc.reshape(B, S, G, dh), pe_k, w_ck1, w_ck2)
    vc = compress_blocks(vc.reshape(B, S, G, dh), pe_v, w_cv1, w_cv2)
    ks = heads(ks, G).reshape(B, G, NBS, SLC_BLOCK, dh)
    vs = heads(vs, G).reshape(B, G, NBS, SLC_BLOCK, dh)
    kw = jnp.pad(heads(kw, G), ((0, 0), (0, 0), (WINDOW, 0), (0, 0)))
    vw = jnp.pad(heads(vw, G), ((0, 0), (0, 0), (WINDOW, 0), (0, 0)))
    gates = jax.nn.sigmoid(gl.astype(jnp.float32)).reshape(B, S, G, NSA_HPG, 3)

    slope_m = alibi_slopes(MOBA_HEADS)
    slope_n = alibi_slopes(NSA_HEADS).reshape(G, NSA_HPG)
    M = cmp_to_slc_matrix(T, NBS)
    hidx = jnp.arange(MOBA_HEADS)[:, None, None]
    gidx = jnp.arange(G)[:, None, None]

    def chunk(i):
        b = i // NC
        s0 = (i % NC) * Q_CHUNK
        t = s0 + jnp.arange(Q_CHUNK)
        take = lambda a: lax.dynamic_index_in_dim(a, b, 0, keepdims=False)

        q = lax.dynamic_slice_in_dim(take(qm), s0, Q_CHUNK, axis=1)
        Kb, Vb, Kmean = take(km), take(vm), take(kmean)
        cur = s0 // MOBA_BLOCK
        gsc = jnp.einsum('hqd,hnd->hqn', q, Kmean).astype(jnp.float32)
        gsc = jnp.where(jnp.arange(NB) < cur, gsc, NEG)
        _, sel = lax.top_k(gsc, KM)
        sel_ok = jnp.arange(KM) < cur
        k_sel = Kb[hidx, sel]
        v_sel = Vb[hidx, sel]
        s_sel = jnp.einsum('hqd,hqkbd->hqkb', q, k_sel).astype(jnp.float32)
        pos_sel = sel[..., None] * MOBA_BLOCK + jnp.arange(MOBA_BLOCK)
        d_sel = (t[None, :, None, None] - pos_sel).astype(jnp.float32)
        s_sel = jnp.where(sel_ok[None, None, :, None], s_sel - slope_m[:, None, None, None] * d_sel, NEG)
        k_own = lax.dynamic_index_in_dim(Kb, cur, 1, keepdims=False)
        v_own = lax.dynamic_index_in_dim(Vb, cur, 1, keepdims=False)
        s_own = jnp.einsum('hqd,hkd->hqk', q, k_own).astype(jnp.float32)
        d_own = t[:, None] - (cur * MOBA_BLOCK + jnp.arange(MOBA_BLOCK))[None, :]
        s_own = jnp.where(d_own >= 0, s_own - slope_m[:, None, None] * d_own.astype(jnp.float32), NEG)
        logits = jnp.concatenate([s_sel.reshape(MOBA_HEADS, Q_CHUNK, KM * MOBA_BLOCK), s_own], axis=-1)
        p = jax.nn.softmax(logits, axis=-1).astype(v_own.dtype)
        p_sel = p[..., :KM * MOBA_BLOCK].reshape(MOBA_HEADS, Q_CHUNK, KM, MOBA_BLOCK)
        o_m = (jnp.einsum('hqkb,hqkbd->hqd', p_sel, v_sel)
               + jnp.einsum('hqb,hbd->hqd', p[..., KM * MOBA_BLOCK:], v_own))
        o_m = o_m.transpose(1, 0, 2).reshape(Q_CHUNK, MOBA_W)

        qg = lax.dynamic_slice_in_dim(take(qn), s0, Q_CHUNK, axis=2)
        kcb, vcb = take(kc), take(vc)
        s_c = jnp.einsum('gjqd,gtd->gjqt', qg, kcb).astype(jnp.float32)
        d_c = t[:, None] - (jnp.arange(T) * CMP_STRIDE + CMP_LEN - 1)[None, :]
        ok_c = d_c >= 0
        s_c = jnp.where(ok_c, s_c - slope_n[:, :, None, None] * d_c.astype(jnp.float32), NEG)
        p_c = jax.nn.softmax(s_c, axis=-1) * ok_c
        o_c = jnp.einsum('gjqt,gtd->gjqd', p_c.astype(vcb.dtype), vcb)
        imp = jnp.einsum('gjqt,tn->gqn', p_c, M)
        blk = t // SLC_BLOCK
        jn = jnp.arange(NBS)[None, :]
        forced = (jn == 0) | (jn == blk[:, None]) | (jn == blk[:, None] - 1)
        imp = jnp.where(forced, FORCE_BONUS, imp)
        imp = jnp.where(jn <= blk[:, None], imp, NEG)
        _, sidx = lax.top_k(imp, NS)
        s_valid = sidx <= blk[None, :, None]
        ksb = take(ks)[gidx, sidx]
        vsb = take(vs)[gidx, sidx]
        s_s = jnp.einsum('gjqd,gqnbd->gjqnb', qg, ksb).astype(jnp.float32)
        d_s = t[None, :, None, None] - (sidx[..., None] * SLC_BLOCK + jnp.arange(SLC_BLOCK))
        ok_s = (d_s >= 0) & s_valid[..., None]
        s_s = jnp.where(ok_s[:, None], s_s - slope_n[:, :, None, None, None] * d_s[:, None].astype(jnp.float32), NEG)
        p_s = jax.nn.softmax(s_s.reshape(G, NSA_HPG, Q_CHUNK, NS * SLC_BLOCK), axis=-1)
        p_s = p_s.reshape(G, NSA_HPG, Q_CHUNK, NS, SLC_BLOCK).astype(vsb.dtype)
        o_s = jnp.einsum('gjqnb,gqnbd->gjqd', p_s, vsb)
        kwb = lax.dynamic_slice_in_dim(take(kw), s0, WINDOW + Q_CHUNK, axis=1)
        vwb = lax.dynamic_slice_in_dim(take(vw), s0, WINDOW + Q_CHUNK, axis=1)
        s_w = jnp.einsum('gjqd,gkd->gjqk', qg, kwb).astype(jnp.float32)
        pos_w = s0 - WINDOW + jnp.arange(WINDOW + Q_CHUNK)
        d_w = t[:, None] - pos_w[None, :]
        ok_w = (d_w >= 0) & (d_w < WINDOW) & (pos_w >= 0)[None, :]
        s_w = jnp.where(ok_w, s_w - slope_n[:, :, None, None] * d_w.astype(jnp.float32), NEG)
        p_w = jax.nn.softmax(s_w, axis=-1).astype(vwb.dtype)
        o_w = jnp.einsum('gjqk,gkd->gjqd', p_w, vwb)
        g = lax.dynamic_slice_in_dim(take(gates), s0, Q_CHUNK, axis=0).transpose(1, 2, 0, 3)
        g = g.astype(o_c.dtype)
        o_n = g[..., 0:1] * o_c + g[..., 1:2] * o_s + g[..., 2:3] * o_w
        o_n = o_n.transpose(2, 0, 1, 3).reshape(Q_CHUNK, NSA_W)
        return o_m, o_n

    o_m, o_n = lax.map(chunk, jnp.arange(B * NC))
    o_m = o_m.reshape(B, S, MOBA_W)
    o_n = o_n.reshape(B, S, NSA_W)
    y = jnp.concatenate([o_m * jax.nn.silu(zm), o_n * jax.nn.silu(zn)], axis=-1)
    return y @ w_out


def setup_inputs(seed: int = 0) -> dict:
    key = jax.random.key(seed)
    ks = jax.random.split(key, 16)
    L, D, dh = DEPTH, D_MODEL, HEAD_DIM
    nrm = lambda k, shape, s: jax.random.normal(k, shape, jnp.float32) * s
    return {
        "x": nrm(ks[0], (BATCH, SEQ, D), 1.0),
        "c": nrm(ks[1], (BATCH, D), 1.0),
        "w_ada": nrm(ks[2], (L, D, 3 * D), D ** -0.5),
        "b_ada": nrm(ks[3], (L, 3 * D), 0.01),
        "g_pre": 1.0 + nrm(ks[4], (L, D), 0.1),
        "g_post": 1.0 + nrm(ks[5], (L, D), 0.1),
        "w_in": nrm(ks[6], (L, D, D_IN), D ** -0.5),
        "w_out": nrm(ks[7], (L, D, D), D ** -0.5),
        "pe_k": nrm(ks[8], (L, CMP_LEN, dh), 0.1),
        "pe_v": nrm(ks[9], (L, CMP_LEN, dh), 0.1),
        "w_ck1": nrm(ks[10], (L, CMP_LEN * dh, dh), (CMP_LEN * dh) ** -0.5),
        "w_ck2": nrm(ks[11], (L, dh, dh), dh ** -0.5),
        "w_cv1": nrm(ks[12], (L, CMP_LEN * dh, dh), (CMP_LEN * dh) ** -0.5),
        "w_cv2": nrm(ks[13], (L, dh, dh), dh ** -0.5),
    }


def reference(x, c, w_ada, b_ada, g_pre, g_post, w_in, w_out, pe_k, pe_v, w_ck1, w_ck2, w_cv1, w_cv2):
    D = x.shape[-1]
    cs = jax.nn.silu(c)
    for l in range(DEPTH):
        mod = cs @ w_ada[l] + b_ada[l]
        shift, scale, gate = mod[:, None, :D], mod[:, None, D:2 * D], mod[:, None, 2 * D:]
        h = rmsnorm(x, g_pre[l]) * (1.0 + scale) + shift
        y = hybrid_mixer(h, w_in[l], w_out[l], pe_k[l], pe_v[l], w_ck1[l], w_ck2[l], w_cv1[l], w_cv2[l])
        x = x + gate * rmsnorm(y, g_post[l])
    return x
```

```python
import contextlib
import numpy as np
import ml_dtypes
import concourse.bass as bass
import concourse.mybir as mybir
from concourse.bass_utils import run_bass_kernel_spmd

F32 = mybir.dt.float32
BF16 = mybir.dt.bfloat16
AF = mybir.ActivationFunctionType
ALU = mybir.AluOpType
AX = mybir.AxisListType

D = 1024
S = 2048
NT = 16
DIN = 3864
NEGM = -30000.0
EPS = 1e-6
N_CORES = 8


class Buf:
    __slots__ = ("w", "r", "name", "excl")

    def __init__(self, name="", excl=False):
        self.w = None
        self.r = {}
        self.name = name
        self.excl = excl


class Sync:
    def __init__(self, nc, es, n_dma_sems=12):
        self.nc = nc
        self.eng = {"pe": nc.tensor, "act": nc.scalar, "dve": nc.vector, "pool": nc.gpsimd, "sp": nc.sync}
        self.sem = {}
        self.cnt = {}
        self.known = {k: {} for k in self.eng}
        for k in ("pe", "act", "dve", "pool"):
            self.sem[k] = es.enter_context(nc.semaphore("s_" + k))
            self.cnt[k] = 0
        self.dsem = []
        for i in range(n_dma_sems):
            k = "d%d" % i
            self.sem[k] = es.enter_context(nc.semaphore("s_" + k))
            self.cnt[k] = 0
            self.dsem.append(k)
        self.dnext = 0
        self.nwaits = 0
        self.nops = 0

    def _wait(self, e, tok):
        k, v = tok
        if self.known[e].get(k, 0) >= v:
            return
        self.known[e][k] = v
        self.eng[e].wait_ge(self.sem[k], v)
        self.nwaits += 1

    def _deps(self, e, reads, writes, defer=False):
        deps = []
        for b in reads:
            if b.w is not None:
                deps.append(b.w)
        for b in writes:
            if b.w is not None:
                deps.append(b.w)
            for k, v in b.r.items():
                deps.append((k, v))
        need = {}
        for k, v in deps:
            if e == "pe" and k == "pe":
                continue
            if self.known[e].get(k, 0) >= v:
                continue
            if need.get(k, 0) < v:
                need[k] = v
        items = list(need.items())
        last = None
        if defer and items:
            last = items.pop()
        for k, v in items:
            self._wait(e, (k, v))
        return last

    def _record(self, tok, reads, writes):
        for b in reads:
            if b.r.get(tok[0], 0) < tok[1]:
                b.r[tok[0]] = tok[1]
        for b in writes:
            b.w = tok
            b.r = {}

    def op(self, e, fn, reads=(), writes=(), attach=True):
        ex = [b for b in reads if b.excl]
        if ex:
            writes = list(writes) + ex
        last = self._deps(e, reads, writes, defer=attach and ATTACH_WAITS)
        ins = fn()
        if last is not None:
            k, v = last
            self.known[e][k] = v
            ins._wait_ge(self.sem[k], v)
        self.cnt[e] += 1
        ins.then_inc(self.sem[e], 1)
        tok = (e, self.cnt[e])
        self._record(tok, reads, writes)
        self.nops += 1
        return tok

    def dma(self, out, in_, reads=(), writes=(), q="sp"):
        k = self.dsem[self.dnext]
        self.dnext = (self.dnext + 1) % len(self.dsem)
        if self.cnt[k] > 0:
            self._wait(q, (k, self.cnt[k]))
        self._deps(q, reads, writes)
        ins = self.eng[q].dma_start(out=out, in_=in_)
        self.cnt[k] += 16
        ins.then_inc(self.sem[k], 16)
        tok = (k, self.cnt[k])
        self._record(tok, reads, writes)
        self.nops += 1
        return tok

    def finish(self):
        for k in self.dsem:
            if self.cnt[k] > 0:
                self._wait("sp", (k, self.cnt[k]))


def _bf(a):
    return np.ascontiguousarray(a.astype(np.float32)).astype(ml_dtypes.bfloat16)


def make_consts():
    c = {}
    p = np.arange(128)[:, None]
    m = np.arange(512)[None, :]
    c["ident"] = _bf(np.eye(128))
    c["identf"] = np.eye(128, dtype=np.float32)
    c["onesf"] = np.ones((128, 128), dtype=np.float32)
    c["tri"] = _bf(np.where((m < 128) & (p > m), NEGM, 0.0))
    i4 = m % 128
    c["tric4"] = _bf(np.where(p > i4, NEGM, 0.0))
    c["tria4"] = _bf(np.where(p <= i4, NEGM, 0.0))
    sh = np.zeros((128, 256), np.float32)
    for u in range(8):
        sh[u, u + 127] = 1.0
    sh[8, 135:] = 1.0
    c["sh"] = _bf(sh)
    tc = np.zeros((128, 512), np.float32)
    for u in range(8):
        tc[u, :] = np.where(i4[0] >= 16 * u + 15, 0.0, NEGM)
    tc[8, :] = NEGM
    c["tc"] = _bf(tc)
    t = (np.arange(NT)[None, :, None] * 128 + np.arange(128)[:, None, None])
    n = np.arange(32)[None, None, :]
    blk = t // 64
    forced = (n == 0) | (n == blk) | (n == blk - 1)
    ctab = np.where(n > blk, -1e9, np.where(forced, 1e4, 0.0)).astype(np.float32)
    c["ctab"] = np.ascontiguousarray(ctab)
    cur = (np.arange(NT) // 2)[None, :, None]
    n8 = np.arange(8)[None, None, :]
    cpad = np.where(n8 == cur, 1e9, np.where(n8 > cur, -1e9, 0.0)).astype(np.float32)
    c["cpad"] = np.ascontiguousarray(np.broadcast_to(cpad, (128, NT, 8)))
    tt = np.arange(S)
    a, b = tt // 64, tt % 64
    slopes = 2.0 ** (-np.arange(1, 9, dtype=np.float64))
    qc = np.zeros((16, 4, S), np.float32)
    for h in range(16):
        s = slopes[h % 8]
        qc[h, 0] = -64.0 * s * a
        qc[h, 1] = -s * b
        qc[h, 2] = 64.0 * s
        qc[h, 3] = s
    c["qcm"] = _bf(qc[:8])
    qn = qc[8:].reshape(2, 4, 4, NT, 128)
    c["qcn"] = _bf(qn.transpose(0, 2, 3, 1, 4).reshape(2, 4, NT * 4 * 128))
    kc = np.stack([np.ones(S), np.ones(S), a, b]).astype(np.float32)
    c["kcon"] = _bf(kc)
    pos = 16 * np.arange(127) + 31
    c["kccon"] = _bf(np.stack([np.ones(127), np.ones(127), pos // 64, pos % 64]))
    c["ind8"] = _bf((tt[None, :] // 256 == np.arange(8)[:, None]))
    c["ind32"] = _bf((tt[None, :] // 64 == np.arange(32)[:, None]))
    i = np.arange(127)[:, None]
    j = np.arange(32)[None, :]
    ov = (i * 16 < (j + 1) * 64) & (i * 16 + 32 > j * 64)
    vcc = np.zeros((127, 33), np.float32)
    vcc[:, 0] = 1.0
    vcc[:, 1:] = ov
    c["vcc"] = _bf(vcc)
    return c


CONST_SHAPES = None


def in_perm():
    offs = np.cumsum([0, 512, 512, 512, 512, 512, 128, 128, 128, 128, 128, 128, 24, 512])
    (oqm, okm, ovm, ozm, oqn, okc, ovc, oks, ovs, okw, ovw, ogl, ozn) = offs[:13]
    perm = []
    for h in range(8):
        for o in (oqm, okm, ovm, ozm):
            perm += list(range(o + 64 * h, o + 64 * h + 64))
    for g in range(2):
        perm += list(range(oqn + 256 * g, oqn + 256 * g + 256))
        for o in (okc, ovc, oks, okw, ovs, ovw):
            perm += list(range(o + 64 * g, o + 64 * g + 64))
        perm += list(range(ogl + 12 * g, ogl + 12 * g + 12))
        perm += list(range(ozn + 256 * g, ozn + 256 * g + 256))
    assert len(perm) == DIN and len(set(perm)) == DIN
    return np.array(perm)


STAGE = 99
ATTACH_WAITS = True
TVAR = 2
SUB = 99


def build(NB, L, consts, debug=False):
    nc = bass.Bass("TRN2", target_bir_lowering=False)

    def din(name, shape, dt=F32):
        return nc.dram_tensor(name, list(shape), dt, kind="ExternalInput").ap()

    x_d = din("x", [NB, S, D])
    cT_d = din("cT", [128, 8, NB])
    wada_d = din("w_ada", [L, D, 3 * D])
    badaT_d = din("b_adaT", [128, L, 24])
    gpreT_d = din("g_preT", [128, L, 8])
    gpostT_d = din("g_postT", [128, L, 8])
    win_d = din("w_in", [L, D, DIN])
    wout_d = din("w_out", [L, D, D])
    wc1_d = {"k": din("wc1k", [L, 64, 2048]), "v": din("wc1v", [L, 64, 2048])}
    peT_d = {"k": din("peTk", [L, 64, 32]), "v": din("peTv", [L, 64, 32])}
    w2_d = {"k": din("w2k", [L, 64, 64]), "v": din("w2v", [L, 64, 64])}
    cd = {}
    for k, v in consts.items():
        cd[k] = din("c_" + k, v.shape, BF16 if v.dtype == ml_dtypes.bfloat16 else F32)
    y_d = nc.dram_tensor("y", [NB, S, D], F32, kind="ExternalOutput").ap()
    if debug:
        dbg_d = nc.dram_tensor("dbg", [S, D], BF16, kind="ExternalOutput").ap()

    with contextlib.ExitStack() as es:
        es.enter_context(nc.allow_low_precision("bf16 matmul operands, fp32 accumulation"))
        sy = Sync(nc, es)
        op, dma = sy.op, sy.dma

        def sb(name, shape, dt):
            return es.enter_context(nc.sbuf_tensor(name, list(shape), dt))

        def ps(name, shape, dt):
            return es.enter_context(nc.psum_tensor(name, list(shape), dt))

        HT = sb("HT", [128, 8, S], BF16);            bHT = [Buf("HT%d" % i) for i in range(4)]
        Y = sb("Y", [128, NT, D], BF16);             bY = [Buf("Y%d" % i) for i in range(NT)]
        ident = sb("ident", [128, 128], BF16);       bC = Buf("consts")
        identf = sb("identf", [128, 128], F32)
        onesf = sb("onesf", [128, 128], F32)
        TRI = sb("TRI", [128, 512], BF16)
        TRIC4 = sb("TRIC4", [128, 512], BF16)
        TRIA4 = sb("TRIA4", [128, 512], BF16)
        SH = sb("SH", [128, 256], BF16)
        TC = sb("TC", [128, 512], BF16)
        CTAB = sb("CTAB", [128, NT, 32], F32)
        CPAD = sb("CPAD", [128, NT, 8], F32)
        NEGH = sb("NEGH", [128, 16], F32)
        WST = [sb("WST%d" % i, [128, 8, 256], F32) for i in range(1)]
        bWST = [Buf("WST0")]
        wst_i = [0]
        WBm = sb("WBm", [128, 8, 256], BF16);        bWBm = Buf("WBm")
        WBn = sb("WBn", [128, 8, 512], BF16);        bWBn = Buf("WBn")
        WC1 = {k: sb("WC1" + k, [64, 32, 64], BF16) for k in "kv"}
        PEb = {k: sb("PE" + k, [64, 32], BF16) for k in "kv"}
        W2 = {k: sb("W2" + k, [64, 64], BF16) for k in "kv"}
        bWC = Buf("WC")
        QA = sb("QA", [128, S], BF16);               bQA = [Buf("QA%d" % i) for i in range(4)]
        KA = sb("KA", [128, S], BF16);               bKA = Buf("KA")
        VA = sb("VA", [128, NT, 65], BF16);          bVA = Buf("VA")
        SZ = sb("SZ", [128, NT, 64], BF16);          bSZ = Buf("SZ")
        KMf = sb("KMf", [64, 8], F32);               bKM = Buf("KM")
        KMb = sb("KMb", [64, 8], BF16)
        GP = sb("GP", [128, NT, 8], F32);            bGP = Buf("GP")
        M8 = sb("M8", [128, 8], F32);                bM8 = Buf("M8")
        M8b = sb("M8b", [128, 8], F32);              bM8b = Buf("M8b")
        MB = sb("MB", [128, NT, 96], BF16);          bMB = [Buf("MB%d" % i) for i in range(4)]
        QAn = sb("QAn", [128, NT * 512], BF16);      bQAn = Buf("QAn")
        KAs = sb("KAs", [128, S], BF16);             bKAs = Buf("KAs")
        KAw = sb("KAw", [128, S], BF16);             bKAw = Buf("KAw")
        VSW = sb("VSW", [128, NT, 2, 65], BF16);     bVSW = Buf("VSW")
        KCA = sb("KCA", [128, 128], BF16);           bKCA = Buf("KCA")
        VCA = sb("VCA", [128, 97], BF16);            bVCA = Buf("VCA")
        KCR1 = sb("KCR", [64, S], BF16)
        KCR = {k: KCR1 for k in "kv"}
        bKCR1 = Buf("KCR")
        bKCR = {k: bKCR1 for k in "kv"}
        SZn = sb("SZn", [128, NT, 256], BF16);       bSZn = Buf("SZn")
        GT = sb("GT", [128, NT, 12], F32);           bGT = Buf("GT")
        OC = sb("OC", [128, NT, 256], BF16);          bOC = [Buf("OC%d" % i) for i in range(NT)]
        IMP = sb("IMP", [128, NT, 32], F32);         bIMP = [Buf("IMP%d" % i) for i in range(NT)]
        TMP32 = sb("TMP32", [128, 32], F32);         bTMP32 = Buf("TMP32")
        XG = sb("XG", [64, 128], F32);               bXG = Buf("XG")
        XG2 = sb("XG2", [64, 128], F32);             bXG2 = Buf("XG2")
        GK = sb("GK", [64, 128], BF16);              bGK = Buf("GK")
        CB = sb("CB", [64, 2], F32);                 bCB = Buf("CB")
        PT = [sb("PT%d" % i, [128, 512], BF16) for i in range(3)]
        bPT = [Buf("PT%d" % i) for i in range(3)]
        pt_i = [0]
        OS = [sb("OS%d" % i, [128, 512], F32) for i in range(2)]
        bOS = [Buf("OS0"), Buf("OS1")]
        os_i = [0]
        XT = [sb("XT%d" % i, [128, D], F32) for i in range(2)]
        bXT = [Buf("XT0"), Buf("XT1")]
        xt_i = [0]
        XN = sb("XN", [128, D], BF16);               bXN = Buf("XN")
        JUNK = sb("JUNK", [128, D], BF16);           bJUNK = Buf("JUNK")
        YT = sb("YT", [128, 8, 128], BF16);          bYT = Buf("YT")
        OT = sb("OT", [128, D], F32);                bOT = Buf("OT")
        GG = sb("GG", [128, D], F32);                bGG = Buf("GG")
        DG = sb("DG", [128, 128], F32);              bDG = Buf("DG")
        TZ = sb("TZ", [128, 256], F32);              bTZ = Buf("TZ")
        SS = sb("SS", [128, NT], F32);               bSS = Buf("SS")
        RSTD = sb("RSTD", [128, NT], F32);           bRSTD = Buf("RSTD")
        SS2 = sb("SS2", [128, 4], F32);              bSS2 = Buf("SS2")
        RS = sb("RS", [128, 8, 1], F32);             bRS = Buf("RS")
        AC = sb("AC", [128, 4, 1], F32);             bAC = Buf("AC")
        TMPF = sb("TMPF", [128, 256], F32);          bTMPF = Buf("TMPF")
        CST = sb("CST", [128, 8, NB], F32);          bCST = Buf("CST")
        CSA = sb("CSA", [128, 8, NB], F32)
        MODT = sb("MODT", [128, L, 24, NB], F32);    bMOD = Buf("MOD")
        BADA = sb("BADA", [128, L, 24], F32)
        GPRE = sb("GPRE", [128, L, 8], F32)
        GPOST = sb("GPOST", [128, L, 8], F32)
        AMOD = sb("AMOD", [128, L, NB, 8], F32)
        GGT = sb("GGT", [128, L, NB, 8], F32)
        SC = [ps("SC%d" % i, [128, 512], F32) for i in range(3)]
        bSC = [Buf("SC%d" % i, True) for i in range(3)]
        sc_i = [0]
        OAB = [ps("OA%d" % i, [128, 512], F32) for i in range(2)]
        bOAB = [Buf("OA0", True), Buf("OA1", True)]
        oa_i = [0]
        PJ = [ps("PJ%d" % i, [128, 512], F32) for i in range(2)]
        bPJ = [Buf("PJ0", True), Buf("PJ1", True)]
        pj_i = [0]
        TP = ps("TP", [128, 1024], BF16);            bTP = Buf("TP", True)

        bYD = [[Buf("YD") for _ in range(NT)] for _ in range(NB)]

        def nxt(lst, bl, ctr):
            i = ctr[0] % len(lst)
            ctr[0] += 1
            return lst[i], bl[i]

        for t_, k in ((ident, "ident"), (identf, "identf"), (onesf, "onesf"), (TRI, "tri"), (TRIC4, "tric4"),
                      (TRIA4, "tria4"), (SH, "sh"), (TC, "tc"), (CTAB, "ctab"), (CPAD, "cpad")):
            dma(t_[:], cd[k], writes=[bC])
        op("pool", lambda: nc.gpsimd.memset(NEGH[:], -0.5), writes=[bC])
        dma(BADA[:], badaT_d, writes=[bC])
        dma(GPRE[:], gpreT_d, writes=[bC])
        dma(GPOST[:], gpostT_d, writes=[bC])
        for t_, b_ in ((QA, bQA), (KA, [bKA]), (KAs, [bKAs]), (KAw, [bKAw]), (KCA, [bKCA]), (QAn, [bQAn])):
            op("pool", lambda t_=t_: nc.gpsimd.memset(t_[:], 0.0), writes=b_)
        op("pool", lambda: nc.gpsimd.memset(MB[:], 0.0), writes=bMB)
        op("pool", lambda: nc.gpsimd.memset(VA[:], 2.0), writes=[bVA])
        op("pool", lambda: nc.gpsimd.memset(VSW[:], 1.0), writes=[bVSW])
        op("pool", lambda: nc.gpsimd.memset(VCA[:], 0.0), writes=[bVCA])
        dma(KA[64:72, :], cd["ind8"], writes=[bKA])
        dma(KA[96:100, :], cd["kcon"], writes=[bKA])
        dma(KAs[64:96, :], cd["ind32"], writes=[bKAs])
        dma(KAs[96:100, :], cd["kcon"], writes=[bKAs])
        dma(KAw[96:100, :], cd["kcon"], writes=[bKAw])
        dma(KCA[96:100, 0:127], cd["kccon"], writes=[bKCA])
        dma(VCA[0:127, 64:97], cd["vcc"], writes=[bVCA])

        def load_cast(dst_ap_fn, src_ap, ncols, bdst, nrow_p=128):
            wst, bw = nxt(WST, bWST, wst_i)
            dma(wst[0:nrow_p, :, 0:ncols], src_ap, writes=[bw])
            op("pool", lambda: nc.gpsimd.tensor_copy(out=dst_ap_fn(), in_=wst[0:nrow_p, :, 0:ncols]),
               reads=[bw], writes=[bdst])

        def silu_parts(out_ap, z_ps_ap, bps, bout, tz_ap):
            op("act", lambda: nc.scalar.activation(out=tz_ap, in_=z_ps_ap, func=AF.Tanh, scale=0.5),
               reads=[bps], writes=[bTZ])
            op("dve", lambda: nc.vector.scalar_tensor_tensor(out=out_ap, in0=tz_ap, scalar=1.0, in1=z_ps_ap,
                                                              op0=ALU.add, op1=ALU.mult),
               reads=[bTZ, bps], writes=[bout])

        def mm(out, lhsT, rhs, start, stop, reads, writes, skip=False):
            op("pe", lambda: nc.tensor.matmul(out, lhsT, rhs, start=start, stop=stop, skip_group_check=skip),
               reads=reads, writes=writes)

        dma(CST[:], cT_d, writes=[bCST])
        op("act", lambda: nc.scalar.activation(out=CSA[:], in_=CST[:], func=AF.Tanh, scale=0.5),
           reads=[bCST], writes=[bMOD])
        op("dve", lambda: nc.vector.scalar_tensor_tensor(out=CSA[:], in0=CSA[:], scalar=1.0, in1=CST[:],
                                                          op0=ALU.add, op1=ALU.mult), reads=[bMOD, bCST], writes=[bMOD])
        op("dve", lambda: nc.vector.tensor_scalar(out=CSA[:], in0=CSA[:], scalar1=0.5, scalar2=None, op0=ALU.mult),
           reads=[bMOD], writes=[bMOD])
        bCSA = bMOD
        for l in range(L):
            for blk in range(12):
                wst, bw = nxt(WST, bWST, wst_i)
                dma(wst[:, :, :], wada_d[l, :, blk * 256:(blk + 1) * 256].rearrange("(kc p) c -> p kc c", p=128),
                    writes=[bw])
                pj, bpj = nxt(PJ, bPJ, pj_i)
                for half in range(2):
                    for kc in range(8):
                        mm(pj[:, half * NB:(half + 1) * NB], wst[:, kc, half * 128:(half + 1) * 128], CSA[:, kc, :],
                           start=(kc == 0), stop=(kc == 7), reads=[bw, bCSA], writes=[bpj],
                           skip=True)
                for half in range(2):
                    j = 2 * blk + half
                    op("dve", lambda half=half, j=j, pj=pj: nc.vector.tensor_scalar(
                        out=MODT[:, l, j, :], in0=pj[:, half * NB:(half + 1) * NB], scalar1=BADA[:, l, j:j + 1],
                        scalar2=None, op0=ALU.add), reads=[bpj, bC], writes=[bMOD])
            for b in range(NB):
                op("dve", lambda b=b, l=l: nc.vector.scalar_tensor_tensor(
                    out=AMOD[:, l, b, :], in0=MODT[:, l, 8:16, b], scalar=1.0, in1=GPRE[:, l, :],
                    op0=ALU.add, op1=ALU.mult), reads=[bMOD, bC], writes=[bMOD])
                op("dve", lambda b=b, l=l: nc.vector.tensor_tensor(
                    out=GGT[:, l, b, :], in0=MODT[:, l, 16:24, b], in1=GPOST[:, l, :], op=ALU.mult),
                    reads=[bMOD, bC], writes=[bMOD])

        def prenorm(b, l, src_d):
            for tt in range(NT):
                xt, bx = nxt(XT, bXT, xt_i)
                dma(xt[:], src_d[b, tt * 128:(tt + 1) * 128, :], reads=[bYD[b][tt]], writes=[bx])
                op("act", lambda xt=xt, tt=tt: nc.scalar.activation(out=JUNK[:], in_=xt[:], func=AF.Square,
                                                                     accum_out=SS[:, tt:tt + 1]),
                   reads=[bx], writes=[bJUNK, bSS], attach=False)
                op("dve", lambda tt=tt: nc.vector.tensor_scalar(out=SS[:, tt:tt + 1], in0=SS[:, tt:tt + 1],
                                                                 scalar1=1.0 / D, scalar2=EPS, op0=ALU.mult, op1=ALU.add),
                   reads=[bSS], writes=[bSS])
                op("pool", lambda tt=tt: nc.gpsimd.tensor_tensor(out=RSTD[:, tt:tt + 1], in0=SS[:, tt:tt + 1],
                                                                  in1=NEGH[:, 0:1], op=ALU.pow),
                   reads=[bSS, bC], writes=[bRSTD])
                op("act", lambda xt=xt, tt=tt: nc.scalar.activation(out=XN[:], in_=xt[:], func=AF.Identity,
                                                                     scale=RSTD[:, tt:tt + 1]),
                   reads=[bx, bRSTD], writes=[bXN])
                for kc in range(8):
                    op("pe", lambda kc=kc: nc.tensor.transpose(out=TP[:, kc * 128:(kc + 1) * 128],
                                                               in_=XN[:, kc * 128:(kc + 1) * 128], identity=ident[:]),
                       reads=[bXN, bC], writes=[bTP])
                for kc in range(8):
                    e = "dve" if kc % 2 == 0 else "act"
                    if e == "dve":
                        op("dve", lambda kc=kc, tt=tt: nc.vector.tensor_scalar(
                            out=HT[:, kc, tt * 128:(tt + 1) * 128], in0=TP[:, kc * 128:(kc + 1) * 128],
                            scalar1=AMOD[:, l, b, kc:kc + 1], scalar2=MODT[:, l, kc, b:b + 1],
                            op0=ALU.mult, op1=ALU.add), reads=[bTP, bMOD], writes=[bHT[tt // 4]])
                    else:
                        op("act", lambda kc=kc, tt=tt: nc.scalar.activation(
                            out=HT[:, kc, tt * 128:(tt + 1) * 128], in_=TP[:, kc * 128:(kc + 1) * 128],
                            func=AF.Identity, scale=AMOD[:, l, b, kc:kc + 1], bias=MODT[:, l, kc, b:b + 1]),
                            reads=[bTP, bMOD], writes=[bHT[tt // 4]])

        def proj_fm(dst_fn, wb, c0, scale, bw, bdst_fn, sview=None):
            for c in range(4):
                pj, bpj = nxt(PJ, bPJ, pj_i)
                for kc in range(8):
                    mm(pj[0:64, :], wb[:, kc, c0:c0 + 64], HT[:, kc, c * 512:(c + 1) * 512],
                       start=(kc == 0), stop=(kc == 7), reads=[bw, bHT[c]], writes=[bpj])
                e = "act" if c % 2 == 0 else "dve"
                src = pj[0:64, :] if sview is None else sview(pj[0:64, :])
                if e == "act":
                    op("act", lambda c=c, src=src: nc.scalar.mul(out=dst_fn(c), in_=src, mul=scale),
                       reads=[bpj], writes=bdst_fn(c))
                else:
                    op("dve", lambda c=c, src=src: nc.vector.tensor_scalar(out=dst_fn(c), in0=src, scalar1=scale,
                                                                            scalar2=None, op0=ALU.mult),
                       reads=[bpj], writes=bdst_fn(c))

        def exp_tile(sc, bsc, rows, c0, c1):
            pt, bpt = nxt(PT, bPT, pt_i)
            op("act", lambda: nc.scalar.activation(out=pt[0:rows, c0:c1], in_=sc[0:rows, c0:c1], func=AF.Exp),
               reads=[bsc], writes=[bpt])
            return pt, bpt

        def acc_to_tok(acc, bacc, nrow, oav, boa):
            os_, bos = nxt(OS, bOS, os_i)
            op("dve", lambda: nc.vector.tensor_copy(out=os_[0:nrow, :], in_=acc[0:nrow, :]), reads=[bacc], writes=[bos])
            for j in range(4):
                op("pe", lambda j=j: nc.tensor.transpose(out=oav[:, j, :], in_=os_[0:nrow, j * 128:(j + 1) * 128],
                                                         identity=identf[0:nrow, 0:nrow]),
                   reads=[bos, bC], writes=[boa])

        def run_tiles(tiles, look=2):
            q = []

            def emit_s(t):
                sc, bsc = nxt(SC, bSC, sc_i)
                n = len(t["s"])
                for k, (lhsT, rhs, rds) in enumerate(t["s"]):
                    mm(sc[0:t["rows"], t["c0"]:t["c1"]], lhsT, rhs, start=(k == 0), stop=(k == n - 1), reads=rds,
                       writes=[bsc])
                return sc, bsc

            def emit_rest(t, sc, bsc):
                pt, bpt = exp_tile(sc, bsc, t["rows"], t["c0"], t["c1"])
                (acc, lhsT_v, st, sp, rds, bacc) = t["pv"]
                mm(acc, lhsT_v, pt[0:t["rows"], t["c0"]:t["c1"]], start=st, stop=sp, reads=[bpt] + rds, writes=[bacc])
                if t.get("after") is not None:
                    t["after"]()

            for t in tiles:
                sc, bsc = emit_s(t)
                q.append((t, sc, bsc))
                if len(q) > look:
                    emit_rest(*q.pop(0))
            while q:
                emit_rest(*q.pop(0))

        def moba_head(b, l, h):
            col0 = 256 * h
            load_cast(lambda: WBm[:, :, :], win_d[l, :, col0:col0 + 256].rearrange("(kc p) c -> p kc c", p=128),
                      256, bWBm)
            dma(QA[96:100, :], cd["qcm"][h], writes=bQA)
            proj_fm(lambda c: QA[0:64, c * 512:(c + 1) * 512], WBm, 0, 0.125, bWBm, lambda c: [bQA[c]])
            proj_fm(lambda c: KA[0:64, c * 512:(c + 1) * 512], WBm, 64, 1.0, bWBm, lambda c: [bKA])
            for g4 in range(4):
                pj, bpj = nxt(PJ, bPJ, pj_i)
                for t4 in range(4):
                    tt = g4 * 4 + t4
                    for kc in range(8):
                        mm(pj[:, t4 * 128:(t4 + 1) * 128], HT[:, kc, tt * 128:(tt + 1) * 128], WBm[:, kc, 128:256],
                           start=(kc == 0), stop=(kc == 7), reads=[bHT[g4], bWBm], writes=[bpj], skip=True)
                pjv = pj[:, :].rearrange("p (t c) -> p t c", c=128)
                op("dve", lambda pjv=pjv, g4=g4: nc.vector.tensor_copy(out=VA[:, g4 * 4:(g4 + 1) * 4, 0:64],
                                                                       in_=pjv[:, :, 0:64]),
                   reads=[bpj], writes=[bVA])
                tzv = TZ[:, 0:256].rearrange("p (t c) -> p t c", c=64)
                silu_parts(SZ[:, g4 * 4:(g4 + 1) * 4, :], pjv[:, :, 64:128], bpj, bSZ, tzv)
            op("dve", lambda: nc.vector.tensor_reduce(out=KMf[:, :], in_=KA[0:64, :].rearrange("p (n k) -> p n k", k=256),
                                                      axis=AX.X, op=ALU.add), reads=[bKA], writes=[bKM])
            op("dve", lambda: nc.vector.tensor_scalar(out=KMb[:, :], in0=KMf[:, :], scalar1=1.0 / 256, scalar2=None,
                                                      op0=ALU.mult), reads=[bKM], writes=[bKM])
            pj, bpj = nxt(PJ, bPJ, pj_i)
            for tt in range(NT):
                mm(pj[:, tt * 8:(tt + 1) * 8], QA[0:64, tt * 128:(tt + 1) * 128], KMb[:, :], start=True, stop=True,
                   reads=[bQA[tt // 4], bKM], writes=[bpj], skip=True)
            op("dve", lambda pj=pj: nc.vector.tensor_tensor(out=GP[:], in0=pj[:, 0:128].rearrange("p (t n) -> p t n", n=8),
                                                            in1=CPAD[:], op=ALU.add), reads=[bpj, bC], writes=[bGP])
            op("dve", lambda: nc.vector.tensor_scalar(out=MB[:, 0:6, 64:72], in0=GP[:, 0:6, :], scalar1=-1e8, scalar2=NEGM,
                                                      op0=ALU.is_lt, op1=ALU.mult), reads=[bGP], writes=[bMB[0], bMB[1]])
            for tt in range(6, NT):
                op("dve", lambda tt=tt: nc.vector.max(out=M8[:, :], in_=GP[:, tt, :]), reads=[bGP], writes=[bM8])
                op("dve", lambda tt=tt: nc.vector.tensor_scalar(out=MB[:, tt, 64:72], in0=GP[:, tt, :], scalar1=M8[:, 3:4],
                                                                 scalar2=NEGM, op0=ALU.is_lt, op1=ALU.mult),
                   reads=[bGP, bM8], writes=[bMB[tt // 4]])
            for c in range(4):
                for t4 in range(4):
                    tt = 4 * c + t4
                    op("pe", lambda tt=tt, t4=t4: nc.tensor.transpose(out=TP[0:96, t4 * 128:(t4 + 1) * 128],
                                                                      in_=MB[:, tt, :], identity=ident[:]),
                       reads=[bMB[c], bC], writes=[bTP])
                op("act", lambda c=c: nc.scalar.copy(out=QA[64:72, c * 512:(c + 1) * 512], in_=TP[64:72, 0:512]),
                   reads=[bTP], writes=[bQA[c]])
            tiles = []
            for c in range(4):
                oa, boa = nxt(OAB, bOAB, oa_i)
                oav = oa[:, 0:260].rearrange("p (j c) -> p j c", c=65)
                nkt = 4 * c + 4

                acc, bacc = nxt(PJ, bPJ, pj_i)

                def epi(c=c, oav=oav, boa=boa, acc=acc, bacc=bacc):
                    acc_to_tok(acc, bacc, 65, oav, boa)
                    op("dve", lambda: nc.vector.reciprocal(out=RS[:, 0:4, :], in_=oav[:, :, 64:65]),
                       reads=[boa], writes=[bRS])
                    for j2 in range(4):
                        tt = 4 * c + j2
                        op("dve", lambda j2=j2, tt=tt: nc.vector.scalar_tensor_tensor(
                            out=Y[:, tt, h * 64:(h + 1) * 64], in0=oav[:, j2, 0:64], scalar=RS[:, j2, :],
                            in1=SZ[:, tt, :], op0=ALU.mult, op1=ALU.mult), reads=[boa, bRS, bSZ], writes=[bY[tt]])

                for kt in range(nkt):
                    j0 = max(0, kt - 4 * c)
                    c0 = 128 * j0
                    diag = kt >= 4 * c
                    smm = [(KA[0:100, kt * 128:(kt + 1) * 128], QA[0:100, c * 512 + c0:(c + 1) * 512], [bKA, bQA[c]])]
                    if diag:
                        smm.append((ident[:, :], TRI[:, 0:512 - c0], [bC]))
                    pv = (acc[0:65, c0:512], VA[:, kt, :], (kt == 0), (kt == nkt - 1), [bVA], bacc)
                    tiles.append(dict(rows=128, c0=c0, c1=512, s=smm, pv=pv, after=epi if kt == nkt - 1 else None))
            run_tiles(tiles)

        def gelu_fm(src_ps, bsrc, bias_ap, out_bf):
            op("act", lambda: nc.scalar.activation(out=XG[:, 0:127], in_=src_ps, func=AF.Identity, bias=bias_ap),
               reads=[bsrc, bCB], writes=[bXG])
            op("dve", lambda: nc.vector.tensor_tensor(out=XG2[:, 0:127], in0=XG[:, 0:127], in1=XG[:, 0:127], op=ALU.mult),
               reads=[bXG], writes=[bXG2])
            op("dve", lambda: nc.vector.tensor_scalar(out=XG2[:, 0:127], in0=XG2[:, 0:127], scalar1=0.044715, scalar2=1.0,
                                                      op0=ALU.mult, op1=ALU.add), reads=[bXG2], writes=[bXG2])
            op("dve", lambda: nc.vector.tensor_tensor(out=XG2[:, 0:127], in0=XG2[:, 0:127], in1=XG[:, 0:127], op=ALU.mult),
               reads=[bXG2, bXG], writes=[bXG2])
            op("act", lambda: nc.scalar.activation(out=XG2[:, 0:127], in_=XG2[:, 0:127], func=AF.Tanh,
                                                   scale=0.7978845608028654), reads=[bXG2], writes=[bXG2])
            op("dve", lambda: nc.vector.scalar_tensor_tensor(out=XG2[:, 0:127], in0=XG2[:, 0:127], scalar=1.0,
                                                              in1=XG[:, 0:127], op0=ALU.add, op1=ALU.mult),
               reads=[bXG2, bXG], writes=[bXG2])
            op("dve", lambda: nc.vector.tensor_scalar(out=out_bf, in0=XG2[:, 0:127], scalar1=0.5, scalar2=None,
                                                      op0=ALU.mult), reads=[bXG2], writes=[bGK])

        def nsa_group(b, l, g):
            col0 = 2048 + 908 * g
            for (a0, a1) in ((0, 256), (256, 512)):
                load_cast(lambda a0=a0, a1=a1: WBn[:, :, a0:a1],
                          win_d[l, :, col0 + a0:col0 + a1].rearrange("(kc p) c -> p kc c", p=128), a1 - a0, bWBn)
            QAv = QAn[:, :].rearrange("p (t j i) -> p t j i", j=4, i=128)

            def compress():
                for kv, wc0 in (("k", 256), ("v", 320)):
                    proj_fm(lambda c, kv=kv: KCR[kv][:, c * 512:(c + 1) * 512], WBn, wc0, 1.0, bWBn,
                            lambda c, kv=kv: [bKCR[kv]])
                    kcr = KCR[kv][:, :].rearrange("p (t r) -> p t r", r=16)
                    pj, bpj = nxt(PJ, bPJ, pj_i)
                    for l_ in range(32):
                        mm(pj[0:64, 200:201], WC1[kv][:, l_, :], PEb[kv][:, l_:l_ + 1], start=(l_ == 0), stop=(l_ == 31),
                           reads=[bWC], writes=[bpj], skip=True)
                    ci = 0 if kv == "k" else 1
                    op("dve", lambda pj=pj, ci=ci: nc.vector.tensor_copy(out=CB[:, ci:ci + 1], in_=pj[0:64, 200:201]),
                       reads=[bpj], writes=[bCB])
                    pj2, bpj2 = nxt(PJ, bPJ, pj_i)
                    for l_ in range(32):
                        rhs = kcr[:, 0:127, l_] if l_ < 16 else kcr[:, 1:128, l_ - 16]
                        mm(pj2[0:64, 0:127], WC1[kv][:, l_, :], rhs, start=(l_ == 0), stop=(l_ == 31),
                           reads=[bWC, bKCR[kv]], writes=[bpj2])
                    gelu_fm(pj2[0:64, 0:127], bpj2, CB[:, ci:ci + 1], GK[:, 0:127])
                    pj3, bpj3 = nxt(PJ, bPJ, pj_i)
                    if kv == "k":
                        mm(pj3[0:64, 0:127], W2["k"][:, :], GK[:, 0:127], start=True, stop=True, reads=[bWC, bGK],
                           writes=[bpj3])
                        op("dve", lambda pj3=pj3: nc.vector.tensor_copy(out=KCA[0:64, 0:127], in_=pj3[0:64, 0:127]),
                           reads=[bpj3], writes=[bKCA])
                    else:
                        mm(pj3[0:127, 0:64], GK[:, 0:127], W2["v"][:, :], start=True, stop=True, reads=[bWC, bGK],
                           writes=[bpj3])
                        op("dve", lambda pj3=pj3: nc.vector.tensor_copy(out=VCA[0:127, 0:64], in_=pj3[0:127, 0:64]),
                           reads=[bpj3], writes=[bVCA])


            dma(QAn[96:100, :], cd["qcn"][g], writes=[bQAn])
            for j in range(4):
                proj_fm(lambda c, j=j: QAv[0:64, 4 * c:4 * c + 4, j, :], WBn, 64 * j, 0.125, bWBn, lambda c: [bQAn],
                        sview=lambda a: a.rearrange("p (t i) -> p t i", i=128))
            proj_fm(lambda c: KAs[0:64, c * 512:(c + 1) * 512], WBn, 384, 1.0, bWBn, lambda c: [bKAs])
            proj_fm(lambda c: KAw[0:64, c * 512:(c + 1) * 512], WBn, 448, 1.0, bWBn, lambda c: [bKAw])
            if SUB < 1:
                return
            compress()
            if SUB < 2:
                return
            for (a0, a1) in ((512, 768), (768, 908)):
                load_cast(lambda a0=a0, a1=a1: WBn[:, :, a0 - 512:a1 - 512],
                          win_d[l, :, col0 + a0:col0 + a1].rearrange("(kc p) c -> p kc c", p=128), a1 - a0, bWBn)
            for tt in range(NT):
                pj, bpj = nxt(PJ, bPJ, pj_i)
                for kc in range(8):
                    mm(pj[:, 0:396], HT[:, kc, tt * 128:(tt + 1) * 128], WBn[:, kc, 0:396],
                       start=(kc == 0), stop=(kc == 7), reads=[bHT[tt // 4], bWBn], writes=[bpj])
                op("dve", lambda pj=pj, tt=tt: nc.vector.tensor_copy(
                    out=VSW[:, tt, :, 0:64], in_=pj[:, 0:128].rearrange("p (a c) -> p a c", c=64)),
                    reads=[bpj], writes=[bVSW])
                op("act", lambda pj=pj, tt=tt: nc.scalar.activation(out=GT[:, tt, :], in_=pj[:, 128:140], func=AF.Tanh,
                                                                     scale=0.5), reads=[bpj], writes=[bGT])
                silu_parts(SZn[:, tt, :], pj[:, 140:396], bpj, bSZn, TZ[:, 0:256])
            op("dve", lambda: nc.vector.tensor_scalar(out=GT[:], in0=GT[:], scalar1=0.5, scalar2=0.5, op0=ALU.mult,
                                                      op1=ALU.add), reads=[bGT], writes=[bGT])
            def branch_epilogue(oav, boa, tt, gi, first, last):
                op("dve", lambda: nc.vector.tensor_scalar(out=RS[:, 0:4, :], in0=oav[:, :, 64:65], scalar1=1e-30,
                                                          scalar2=None, op0=ALU.add), reads=[boa], writes=[bRS])
                op("dve", lambda: nc.vector.reciprocal(out=RS[:, 0:4, :], in_=RS[:, 0:4, :]), reads=[bRS], writes=[bRS])
                gv = GT[:, tt, :].rearrange("p (j r) -> p j r", r=3)
                op("dve", lambda: nc.vector.tensor_tensor(out=AC[:, :, :], in0=RS[:, 0:4, :], in1=gv[:, :, gi:gi + 1],
                                                          op=ALU.mult), reads=[bRS, bGT], writes=[bAC])
                ocv = OC[:, tt, :].rearrange("p (j c) -> p j c", c=64)
                for j in range(4):
                    if first:
                        op("dve", lambda j=j: nc.vector.tensor_scalar(out=ocv[:, j, :], in0=oav[:, j, 0:64],
                                                                      scalar1=AC[:, j, :], scalar2=None, op0=ALU.mult),
                           reads=[boa, bAC], writes=[bOC[tt]])
                    else:
                        op("dve", lambda j=j: nc.vector.scalar_tensor_tensor(
                            out=ocv[:, j, :], in0=oav[:, j, 0:64], scalar=AC[:, j, :], in1=ocv[:, j, :],
                            op0=ALU.mult, op1=ALU.add), reads=[boa, bAC, bOC[tt]], writes=[bOC[tt]])
                if last:
                    yc = 512 + 256 * g
                    op("dve", lambda: nc.vector.scalar_tensor_tensor(
                        out=Y[:, tt, yc:yc + 256], in0=OC[:, tt, :], scalar=0.5, in1=SZn[:, tt, :],
                        op0=ALU.mult, op1=ALU.mult), reads=[bOC[tt], bSZn], writes=[bY[tt]])

            if SUB < 3:
                return
            tiles = []
            for tt in range(NT):
                oa, boa = nxt(OAB, bOAB, oa_i)
                oav = oa[:, 0:388].rearrange("p (j c) -> p j c", c=97)

                acc, bacc = nxt(PJ, bPJ, pj_i)

                def epiA(tt=tt, oav=oav, boa=boa, acc=acc, bacc=bacc):
                    acc_to_tok(acc, bacc, 97, oav, boa)
                    branch_epilogue(oav, boa, tt, 0, True, False)
                    for j in range(4):
                        in1 = CTAB[:, tt, :] if j == 0 else IMP[:, tt, :]
                        op("dve", lambda j=j, in1=in1: nc.vector.scalar_tensor_tensor(
                            out=IMP[:, tt, :], in0=oav[:, j, 65:97], scalar=RS[:, j, :], in1=in1,
                            op0=ALU.mult, op1=ALU.add), reads=[boa, bRS, bC, bIMP[tt]], writes=[bIMP[tt]])
                    if tt >= 8:
                        op("dve", lambda: nc.vector.max(out=M8[:, :], in_=IMP[:, tt, :]), reads=[bIMP[tt]], writes=[bM8])
                        op("dve", lambda: nc.vector.match_replace(out=TMP32[:, :], in_to_replace=M8[:, :],
                                                                  in_values=IMP[:, tt, :], imm_value=-3e38),
                           reads=[bM8, bIMP[tt]], writes=[bTMP32], attach=False)
                        op("dve", lambda: nc.vector.max(out=M8b[:, :], in_=TMP32[:, :]), reads=[bTMP32], writes=[bM8b])
                        op("dve", lambda: nc.vector.tensor_scalar(out=MB[:, tt, 64:96], in0=IMP[:, tt, :],
                                                                  scalar1=M8b[:, 7:8], scalar2=NEGM, op0=ALU.is_lt,
                                                                  op1=ALU.mult), reads=[bIMP[tt], bM8b],
                           writes=[bMB[tt // 4]])
                    else:
                        op("dve", lambda: nc.vector.tensor_scalar(out=MB[:, tt, 64:96], in0=IMP[:, tt, :], scalar1=-1e8,
                                                                  scalar2=NEGM, op0=ALU.is_lt, op1=ALU.mult),
                           reads=[bIMP[tt]], writes=[bMB[tt // 4]])

                smm = [(KCA[0:100, 0:127], QAn[0:100, tt * 512:(tt + 1) * 512], [bKCA, bQAn]),
                       (SH[:, 128 - 8 * tt:128 - 8 * tt + 127], TC[:, :], [bC])]
                pv = (acc[0:97, 0:512], VCA[0:127, :], True, True, [bVCA], bacc)
                tiles.append(dict(rows=127, c0=0, c1=512, s=smm, pv=pv, after=epiA))
            if SUB < 4:
                run_tiles(tiles)
                return
            for tt in range(NT):
                oa, boa = nxt(OAB, bOAB, oa_i)
                oav = oa[:, 0:260].rearrange("p (j c) -> p j c", c=65)
                kts = [kt for kt in range(tt - 4, tt + 1) if kt >= 0]

                acc, bacc = nxt(PJ, bPJ, pj_i)

                def epiW(tt=tt, oav=oav, boa=boa, acc=acc, bacc=bacc):
                    acc_to_tok(acc, bacc, 65, oav, boa)
                    branch_epilogue(oav, boa, tt, 2, False, False)

                for kt in kts:
                    extra = TRIC4 if kt == tt else (TRIA4 if kt == tt - 4 else None)
                    smm = [(KAw[0:100, kt * 128:(kt + 1) * 128], QAn[0:100, tt * 512:(tt + 1) * 512], [bKAw, bQAn])]
                    if extra is not None:
                        smm.append((ident[:, :], extra[:, :], [bC]))
                    pv = (acc[0:65, 0:512], VSW[:, kt, 1, :], (kt == kts[0]), (kt == kts[-1]), [bVSW], bacc)
                    tiles.append(dict(rows=128, c0=0, c1=512, s=smm, pv=pv, after=epiW if kt == kts[-1] else None))
            run_tiles(tiles)
            if SUB < 5:
                return
            for c in range(4):
                for t4 in range(4):
                    tt = 4 * c + t4
                    op("pe", lambda tt=tt, t4=t4: nc.tensor.transpose(out=TP[0:96, t4 * 128:(t4 + 1) * 128],
                                                                      in_=MB[:, tt, :], identity=ident[:]),
                       reads=[bMB[c], bC], writes=[bTP])
                tpv = TP[64:96, 0:512].rearrange("p (t i) -> p t i", i=128)
                if TVAR == 1:
                    continue
                for j in range(4):
                    e = "act" if (j % 2 == 0 or TVAR == 2) else "dve"
                    if e == "act":
                        op("act", lambda j=j, c=c, tpv=tpv: nc.scalar.copy(out=QAv[64:96, 4 * c:4 * c + 4, j, :], in_=tpv),
                           reads=[bTP], writes=[bQAn])
                    else:
                        op("dve", lambda j=j, c=c, tpv=tpv: nc.vector.tensor_copy(out=QAv[64:96, 4 * c:4 * c + 4, j, :],
                                                                                  in_=tpv), reads=[bTP], writes=[bQAn])
            if SUB < 6:
                return
            tiles = []
            for tt in range(NT):
                oa, boa = nxt(OAB, bOAB, oa_i)
                oav = oa[:, 0:260].rearrange("p (j c) -> p j c", c=65)

                acc, bacc = nxt(PJ, bPJ, pj_i)

                def epiS(tt=tt, oav=oav, boa=boa, acc=acc, bacc=bacc):
                    acc_to_tok(acc, bacc, 65, oav, boa)
                    branch_epilogue(oav, boa, tt, 1, False, True)

                for kt in range(tt + 1):
                    smm = [(KAs[0:100, kt * 128:(kt + 1) * 128], QAn[0:100, tt * 512:(tt + 1) * 512], [bKAs, bQAn])]
                    if kt == tt:
                        smm.append((ident[:, :], TRIC4[:, :], [bC]))
                    pv = (acc[0:65, 0:512], VSW[:, kt, 0, :], (kt == 0), (kt == tt), [bVSW], bacc)
                    tiles.append(dict(rows=128, c0=0, c1=512, s=smm, pv=pv, after=epiS if kt == tt else None))
            run_tiles(tiles)

        def outproj(b, l, src_d):
            WO = QAn[:, :].rearrange("p (kc c) -> p kc c", c=1024)
            for blk in range(4):
                load_cast(lambda blk=blk: WO[:, :, blk * 256:(blk + 1) * 256],
                          wout_d[l, :, blk * 256:(blk + 1) * 256].rearrange("(kc p) c -> p kc c", p=128), 256, bQAn)
            for kc in range(8):
                op("dve", lambda kc=kc: nc.vector.tensor_scalar(out=DG[:, :], in0=identf[:, :],
                                                                scalar1=GGT[:, l, b, kc:kc + 1], scalar2=None,
                                                                op0=ALU.mult), reads=[bC, bMOD], writes=[bDG])
                pj, bpj = nxt(PJ, bPJ, pj_i)
                mm(pj[:, 0:128], onesf[:, :], DG[:, :], start=True, stop=True, reads=[bC, bDG], writes=[bpj])
                op("act", lambda kc=kc, pj=pj: nc.scalar.copy(out=GG[:, kc * 128:(kc + 1) * 128], in_=pj[:, 0:128]),
                   reads=[bpj], writes=[bGG])
            for tt in range(NT):
                for kc in range(8):
                    op("pe", lambda kc=kc, tt=tt: nc.tensor.transpose(out=TP[:, kc * 128:(kc + 1) * 128],
                                                                      in_=Y[:, tt, kc * 128:(kc + 1) * 128],
                                                                      identity=ident[:]),
                       reads=[bY[tt], bC], writes=[bTP])
                op("act", lambda: nc.scalar.copy(out=YT[:, 0:4, :], in_=TP[:, 0:512].rearrange("p (k i) -> p k i", i=128)),
                   reads=[bTP], writes=[bYT])
                op("dve", lambda: nc.vector.tensor_copy(out=YT[:, 4:8, :],
                                                        in_=TP[:, 512:1024].rearrange("p (k i) -> p k i", i=128)),
                   reads=[bTP], writes=[bYT])
                xt, bx = nxt(XT, bXT, xt_i)
                dma(xt[:], src_d[b, tt * 128:(tt + 1) * 128, :], reads=[bYD[b][tt]], writes=[bx])
                pjs = []
                for half in range(2):
                    pj, bpj = nxt(PJ, bPJ, pj_i)
                    for kc in range(8):
                        mm(pj[:, :], YT[:, kc, :], WO[:, kc, half * 512:(half + 1) * 512], start=(kc == 0),
                           stop=(kc == 7), reads=[bYT, bQAn], writes=[bpj])
                    op("act", lambda pj=pj, half=half: nc.scalar.activation(
                        out=JUNK[:, half * 512:(half + 1) * 512], in_=pj[:, :], func=AF.Square,
                        accum_out=SS2[:, half:half + 1]), reads=[bpj], writes=[bJUNK, bSS2], attach=False)
                    pjs.append((pj, bpj))
                op("dve", lambda: nc.vector.tensor_tensor(out=SS2[:, 2:3], in0=SS2[:, 0:1], in1=SS2[:, 1:2], op=ALU.add),
                   reads=[bSS2], writes=[bSS2])
                op("dve", lambda: nc.vector.tensor_scalar(out=SS2[:, 2:3], in0=SS2[:, 2:3], scalar1=1.0 / D, scalar2=EPS,
                                                          op0=ALU.mult, op1=ALU.add), reads=[bSS2], writes=[bSS2])
                op("pool", lambda: nc.gpsimd.tensor_tensor(out=SS2[:, 3:4], in0=SS2[:, 2:3], in1=NEGH[:, 0:1], op=ALU.pow),
                   reads=[bSS2, bC], writes=[bSS2])
                for half in range(2):
                    pj, bpj = pjs[half]
                    sl = slice(half * 512, (half + 1) * 512)
                    op("dve", lambda pj=pj, sl=sl: nc.vector.scalar_tensor_tensor(
                        out=OT[:, sl], in0=pj[:, :], scalar=SS2[:, 3:4], in1=GG[:, sl], op0=ALU.mult, op1=ALU.mult),
                        reads=[bpj, bSS2, bGG], writes=[bOT])
                op("pool", lambda xt=xt: nc.gpsimd.tensor_tensor(out=OT[:, :], in0=OT[:, :], in1=xt[:, :], op=ALU.add),
                   reads=[bOT, bx], writes=[bOT])
                dma(y_d[b, tt * 128:(tt + 1) * 128, :], OT[:, :], reads=[bOT], writes=[bYD[b][tt]])

        for b in range(NB):
            for l in range(L):
                src = x_d if l == 0 else y_d
                if STAGE < 2:
                    continue
                prenorm(b, l, src)
                if STAGE < 3:
                    continue
                for kv in "kv":
                    wst, bw = nxt(WST, bWST, wst_i)
                    wv = wst[0:64, :, :].rearrange("p a c -> p (a c)")
                    dma(wv, wc1_d[kv][l], writes=[bw])
                    op("pool", lambda kv=kv, wv=wv: nc.gpsimd.tensor_copy(
                        out=WC1[kv][:, :, :].rearrange("p a c -> p (a c)"), in_=wv), reads=[bw], writes=[bWC])
                    wst, bw = nxt(WST, bWST, wst_i)
                    wv2 = wst[0:64, 0, 0:32]
                    wv3 = wst[0:64, 1, 0:64]
                    dma(wv2, peT_d[kv][l], writes=[bw])
                    dma(wv3, w2_d[kv][l], writes=[bw])
                    op("pool", lambda kv=kv, wv2=wv2: nc.gpsimd.tensor_copy(out=PEb[kv][:, :], in_=wv2), reads=[bw],
                       writes=[bWC])
                    op("pool", lambda kv=kv, wv3=wv3: nc.gpsimd.tensor_copy(out=W2[kv][:, :], in_=wv3), reads=[bw],
                       writes=[bWC])
                for h in range(8 if STAGE >= 5 else (1 if STAGE == 4 else 0)):
                    moba_head(b, l, h)
                for g in range(2 if STAGE >= 7 else (1 if STAGE == 6 else 0)):
                    nsa_group(b, l, g)
                if STAGE < 8:
                    continue
                if debug and b == 0 and l == 0:
                    for tt in range(NT):
                        dma(dbg_d[tt * 128:(tt + 1) * 128, :], Y[:, tt, :], reads=[bY[tt]])
                outproj(b, l, src)
        sy.finish()
        build.stats = (sy.nops, sy.nwaits)
    return nc


def prep_shared(inputs, L):
    f = lambda a: np.ascontiguousarray(np.asarray(a, dtype=np.float32))
    perm = in_perm()
    sh = {}
    sh["w_ada"] = f(inputs["w_ada"][:L])
    sh["b_adaT"] = f(np.asarray(inputs["b_ada"][:L]).reshape(L, 24, 128).transpose(2, 0, 1))
    sh["g_preT"] = f(np.asarray(inputs["g_pre"][:L]).reshape(L, 8, 128).transpose(2, 0, 1))
    sh["g_postT"] = f(np.asarray(inputs["g_post"][:L]).reshape(L, 8, 128).transpose(2, 0, 1))
    sh["w_in"] = f(np.asarray(inputs["w_in"][:L])[:, :, perm])
    sh["w_out"] = f(inputs["w_out"][:L])
    for kv, n1, n2, npe in (("k", "w_ck1", "w_ck2", "pe_k"), ("v", "w_cv1", "w_cv2", "pe_v")):
        w1 = np.asarray(inputs[n1][:L]).reshape(L, 32, 64, 64).transpose(0, 2, 1, 3).reshape(L, 64, 2048)
        sh["wc1" + kv] = f(w1)
        sh["peT" + kv] = f(np.asarray(inputs[npe][:L]).transpose(0, 2, 1))
        sh["w2" + kv] = f(inputs[n2][:L])
    return sh


_CACHE = {}


def run(inputs, n_cores, NB, L, debug=False):
    consts = make_consts()
    key = (NB, L, debug)
    if key not in _CACHE:
        _CACHE[key] = build(NB, L, consts, debug=debug)
    nc = _CACHE[key]
    sh = prep_shared(inputs, L)
    for k, v in consts.items():
        sh["c_" + k] = v
    x = np.asarray(inputs["x"], dtype=np.float32)
    c = np.asarray(inputs["c"], dtype=np.float32)
    in_maps = []
    for i in range(n_cores):
        m = dict(sh)
        m["x"] = np.ascontiguousarray(x[i * NB:(i + 1) * NB])
        ci = c[i * NB:(i + 1) * NB]
        m["cT"] = np.ascontiguousarray(ci.reshape(NB, 8, 128).transpose(2, 1, 0))
        in_maps.append(m)
    res = run_bass_kernel_spmd(nc, in_maps, core_ids=list(range(n_cores)))
    out = np.concatenate([np.asarray(r["y"]) for r in res.results], axis=0)
    if debug:
        return out, np.asarray(res.results[0]["dbg"])
    return out


def kernel(x, c, w_ada, b_ada, g_pre, g_post, w_in, w_out, pe_k, pe_v, w_ck1, w_ck2, w_cv1, w_cv2):
    inputs = dict(x=x, c=c, w_ada=w_ada, b_ada=b_ada, g_pre=g_pre, g_post=g_post, w_in=w_in, w_out=w_out,
                  pe_k=pe_k, pe_v=pe_v, w_ck1=w_ck1, w_ck2=w_ck2, w_cv1=w_cv1, w_cv2=w_cv2)
    NB = x.shape[0] // N_CORES
    out = run(inputs, N_CORES, NB, 2)
    return out.astype(np.float32, copy=False)
```

```python
import contextlib
import numpy as np
import ml_dtypes
import concourse.bass as bass
import concourse.mybir as mybir
from concourse.bass_utils import run_bass_kernel_spmd

F32 = mybir.dt.float32
BF16 = mybir.dt.bfloat16
AF = mybir.ActivationFunctionType
ALU = mybir.AluOpType
AX = mybir.AxisListType

D = 1024
S = 2048
NT = 16
DIN = 3864
NEGM = -30000.0
EPS = 1e-6
N_CORES = 8


class Buf:
    __slots__ = ("w", "r", "name", "excl")

    def __init__(self, name="", excl=False):
        self.w = None
        self.r = {}
        self.name = name
        self.excl = excl


class Sync:
    def __init__(self, nc, es, n_dma_sems=12):
        self.nc = nc
        self.eng = {"pe": nc.tensor, "act": nc.scalar, "dve": nc.vector, "pool": nc.gpsimd, "sp": nc.sync}
        self.sem = {}
        self.cnt = {}
        self.known = {k: {} for k in self.eng}
        for k in ("pe", "act", "dve", "pool"):
            self.sem[k] = es.enter_context(nc.semaphore("s_" + k))
            self.cnt[k] = 0
        self.dsem = []
        for i in range(n_dma_sems):
            k = "d%d" % i
            self.sem[k] = es.enter_context(nc.semaphore("s_" + k))
            self.cnt[k] = 0
            self.dsem.append(k)
        self.dnext = 0
        self.nwaits = 0
        self.nops = 0

    def _wait(self, e, tok):
        k, v = tok
        if self.known[e].get(k, 0) >= v:
            return
        self.known[e][k] = v
        self.eng[e].wait_ge(self.sem[k], v)
        self.nwaits += 1

    def _deps(self, e, reads, writes, defer=False):
        deps = []
        for b in reads:
            if b.w is not None:
                deps.append(b.w)
        for b in writes:
            if b.w is not None:
                deps.append(b.w)
            for k, v in b.r.items():
                deps.append((k, v))
        need = {}
        for k, v in deps:
            if e == "pe" and k == "pe":
                continue
            if self.known[e].get(k, 0) >= v:
                continue
            if need.get(k, 0) < v:
                need[k] = v
        items = list(need.items())
        last = None
        if defer and items:
            last = items.pop()
        for k, v in items:
            self._wait(e, (k, v))
        return last

    def _record(self, tok, reads, writes):
        for b in reads:
            if b.r.get(tok[0], 0) < tok[1]:
                b.r[tok[0]] = tok[1]
        for b in writes:
            b.w = tok
            b.r = {}

    def op(self, e, fn, reads=(), writes=(), attach=True):
        ex = [b for b in reads if b.excl]
        if ex:
            writes = list(writes) + ex
        last = self._deps(e, reads, writes, defer=attach and ATTACH_WAITS)
        ins = fn()
        if last is not None:
            k, v = last
            self.known[e][k] = v
            ins._wait_ge(self.sem[k], v)
        self.cnt[e] += 1
        ins.then_inc(self.sem[e], 1)
        tok = (e, self.cnt[e])
        self._record(tok, reads, writes)
        self.nops += 1
        return tok

    def dma(self, out, in_, reads=(), writes=(), q="sp"):
        k = self.dsem[self.dnext]
        self.dnext = (self.dnext + 1) % len(self.dsem)
        if self.cnt[k] > 0:
            self._wait(q, (k, self.cnt[k]))
        self._deps(q, reads, writes)
        ins = self.eng[q].dma_start(out=out, in_=in_)
        self.cnt[k] += 16
        ins.then_inc(self.sem[k], 16)
        tok = (k, self.cnt[k])
        self._record(tok, reads, writes)
        self.nops += 1
        return tok

    def finish(self):
        for k in self.dsem:
            if self.cnt[k] > 0:
                self._wait("sp", (k, self.cnt[k]))


def _bf(a):
    return np.ascontiguousarray(a.astype(np.float32)).astype(ml_dtypes.bfloat16)


def make_consts():
    c = {}
    p = np.arange(128)[:, None]
    m = np.arange(512)[None, :]
    c["ident"] = _bf(np.eye(128))
    c["identf"] = np.eye(128, dtype=np.float32)
    c["onesf"] = np.ones((128, 128), dtype=np.float32)
    c["tri"] = _bf(np.where((m < 128) & (p > m), NEGM, 0.0))
    i4 = m % 128
    c["tric4"] = _bf(np.where(p > i4, NEGM, 0.0))
    c["tria4"] = _bf(np.where(p <= i4, NEGM, 0.0))
    sh = np.zeros((128, 256), np.float32)
    for u in range(8):
        sh[u, u + 127] = 1.0
    sh[8, 135:] = 1.0
    c["sh"] = _bf(sh)
    tc = np.zeros((128, 512), np.float32)
    for u in range(8):
        tc[u, :] = np.where(i4[0] >= 16 * u + 15, 0.0, NEGM)
    tc[8, :] = NEGM
    c["tc"] = _bf(tc)
    t = (np.arange(NT)[None, :, None] * 128 + np.arange(128)[:, None, None])
    n = np.arange(32)[None, None, :]
    blk = t // 64
    forced = (n == 0) | (n == blk) | (n == blk - 1)
    ctab = np.where(n > blk, -1e9, np.where(forced, 1e4, 0.0)).astype(np.float32)
    c["ctab"] = np.ascontiguousarray(ctab)
    cur = (np.arange(NT) // 2)[None, :, None]
    n8 = np.arange(8)[None, None, :]
    cpad = np.where(n8 == cur, 1e9, np.where(n8 > cur, -1e9, 0.0)).astype(np.float32)
    c["cpad"] = np.ascontiguousarray(np.broadcast_to(cpad, (128, NT, 8)))
    tt = np.arange(S)
    a, b = tt // 64, tt % 64
    slopes = 2.0 ** (-np.arange(1, 9, dtype=np.float64))
    qc = np.zeros((16, 4, S), np.float32)
    for h in range(16):
        s = slopes[h % 8]
        qc[h, 0] = -64.0 * s * a
        qc[h, 1] = -s * b
        qc[h, 2] = 64.0 * s
        qc[h, 3] = s
    c["qcm"] = _bf(qc[:8])
    qn = qc[8:].reshape(2, 4, 4, NT, 128)
    c["qcn"] = _bf(qn.transpose(0, 2, 3, 1, 4).reshape(2, 4, NT * 4 * 128))
    kc = np.stack([np.ones(S), np.ones(S), a, b]).astype(np.float32)
    c["kcon"] = _bf(kc)
    pos = 16 * np.arange(127) + 31
    c["kccon"] = _bf(np.stack([np.ones(127), np.ones(127), pos // 64, pos % 64]))
    c["ind8"] = _bf((tt[None, :] // 256 == np.arange(8)[:, None]))
    c["ind32"] = _bf((tt[None, :] // 64 == np.arange(32)[:, None]))
    i = np.arange(127)[:, None]
    j = np.arange(32)[None, :]
    ov = (i * 16 < (j + 1) * 64) & (i * 16 + 32 > j * 64)
    vcc = np.zeros((127, 33), np.float32)
    vcc[:, 0] = 1.0
    vcc[:, 1:] = ov
    c["vcc"] = _bf(vcc)
    return c


CONST_SHAPES = None


def in_perm():
    offs = np.cumsum([0, 512, 512, 512, 512, 512, 128, 128, 128, 128, 128, 128, 24, 512])
    (oqm, okm, ovm, ozm, oqn, okc, ovc, oks, ovs, okw, ovw, ogl, ozn) = offs[:13]
    perm = []
    for h in range(8):
        for o in (oqm, okm, ovm, ozm):
            perm += list(range(o + 64 * h, o + 64 * h + 64))
    for g in range(2):
        perm += list(range(oqn + 256 * g, oqn + 256 * g + 256))
        for o in (okc, ovc, oks, okw, ovs, ovw):
            perm += list(range(o + 64 * g, o + 64 * g + 64))
        perm += list(range(ogl + 12 * g, ogl + 12 * g + 12))
        perm += list(range(ozn + 256 * g, ozn + 256 * g + 256))
    assert len(perm) == DIN and len(set(perm)) == DIN
    return np.array(perm)


STAGE = 99
ATTACH_WAITS = True
TVAR = 2
SUB = 99


def build(NB, L, consts, debug=False):
    nc = bass.Bass("TRN2", target_bir_lowering=False)

    def din(name, shape, dt=F32):
        return nc.dram_tensor(name, list(shape), dt, kind="ExternalInput").ap()

    x_d = din("x", [NB, S, D])
    cT_d = din("cT", [128, 8, NB])
    wada_d = din("w_ada", [L, D, 3 * D])
    badaT_d = din("b_adaT", [128, L, 24])
    gpreT_d = din("g_preT", [128, L, 8])
    gpostT_d = din("g_postT", [128, L, 8])
    win_d = din("w_in", [L, D, DIN])
    wout_d = din("w_out", [L, D, D])
    wc1_d = {"k": din("wc1k", [L, 64, 2048]), "v": din("wc1v", [L, 64, 2048])}
    peT_d = {"k": din("peTk", [L, 64, 32]), "v": din("peTv", [L, 64, 32])}
    w2_d = {"k": din("w2k", [L, 64, 64]), "v": din("w2v", [L, 64, 64])}
    cd = {}
    for k, v in consts.items():
        cd[k] = din("c_" + k, v.shape, BF16 if v.dtype == ml_dtypes.bfloat16 else F32)
    y_d = nc.dram_tensor("y", [NB, S, D], F32, kind="ExternalOutput").ap()
    if debug:
        dbg_d = nc.dram_tensor("dbg", [S, D], BF16, kind="ExternalOutput").ap()

    with contextlib.ExitStack() as es:
        es.enter_context(nc.allow_low_precision("bf16 matmul operands, fp32 accumulation"))
        sy = Sync(nc, es)
        op, dma = sy.op, sy.dma

        def sb(name, shape, dt):
            return es.enter_context(nc.sbuf_tensor(name, list(shape), dt))

        def ps(name, shape, dt):
            return es.enter_context(nc.psum_tensor(name, list(shape), dt))

        HT = sb("HT", [128, 8, S], BF16);            bHT = [Buf("HT%d" % i) for i in range(4)]
        Y = sb("Y", [128, NT, D], BF16);             bY = [Buf("Y%d" % i) for i in range(NT)]
        ident = sb("ident", [128, 128], BF16);       bC = Buf("consts")
        identf = sb("identf", [128, 128], F32)
        onesf = sb("onesf", [128, 128], F32)
        TRI = sb("TRI", [128, 512], BF16)
        TRIC4 = sb("TRIC4", [128, 512], BF16)
        TRIA4 = sb("TRIA4", [128, 512], BF16)
        SH = sb("SH", [128, 256], BF16)
        TC = sb("TC", [128, 512], BF16)
        CTAB = sb("CTAB", [128, NT, 32], F32)
        CPAD = sb("CPAD", [128, NT, 8], F32)
        NEGH = sb("NEGH", [128, 16], F32)
        WST = [sb("WST%d" % i, [128, 8, 256], F32) for i in range(1)]
        bWST = [Buf("WST0")]
        wst_i = [0]
        WBm = sb("WBm", [128, 8, 256], BF16);        bWBm = Buf("WBm")
        WBn = sb("WBn", [128, 8, 512], BF16);        bWBn = Buf("WBn")
        WC1 = {k: sb("WC1" + k, [64, 32, 64], BF16) for k in "kv"}
        PEb = {k: sb("PE" + k, [64, 32], BF16) for k in "kv"}
        W2 = {k: sb("W2" + k, [64, 64], BF16) for k in "kv"}
        bWC = Buf("WC")
        QA = sb("QA", [128, S], BF16);               bQA = [Buf("QA%d" % i) for i in range(4)]
        KA = sb("KA", [128, S], BF16);               bKA = Buf("KA")
        VA = sb("VA", [128, NT, 65], BF16);          bVA = Buf("VA")
        SZ = sb("SZ", [128, NT, 64], BF16);          bSZ = Buf("SZ")
        KMf = sb("KMf", [64, 8], F32);               bKM = Buf("KM")
        KMb = sb("KMb", [64, 8], BF16)
        GP = sb("GP", [128, NT, 8], F32);            bGP = Buf("GP")
        M8 = sb("M8", [128, 8], F32);                bM8 = Buf("M8")
        M8b = sb("M8b", [128, 8], F32);              bM8b = Buf("M8b")
        MB = sb("MB", [128, NT, 96], BF16);          bMB = [Buf("MB%d" % i) for i in range(4)]
        QAn = sb("QAn", [128, NT * 512], BF16);      bQAn = Buf("QAn")
        KAs = sb("KAs", [128, S], BF16);             bKAs = Buf("KAs")
        KAw = sb("KAw", [128, S], BF16);             bKAw = Buf("KAw")
        VSW = sb("VSW", [128, NT, 2, 65], BF16);     bVSW = Buf("VSW")
        KCA = sb("KCA", [128, 128], BF16);           bKCA = Buf("KCA")
        VCA = sb("VCA", [128, 97], BF16);            bVCA = Buf("VCA")
        KCR1 = sb("KCR", [64, S], BF16)
        KCR = {k: KCR1 for k in "kv"}
        bKCR1 = Buf("KCR")
        bKCR = {k: bKCR1 for k in "kv"}
        SZn = sb("SZn", [128, NT, 256], BF16);       bSZn = Buf("SZn")
        GT = sb("GT", [128, NT, 12], F32);           bGT = Buf("GT")
        OC = sb("OC", [128, NT, 256], BF16);          bOC = [Buf("OC%d" % i) for i in range(NT)]
        IMP = sb("IMP", [128, NT, 32], F32);         bIMP = [Buf("IMP%d" % i) for i in range(NT)]
        TMP32 = sb("TMP32", [128, 32], F32);         bTMP32 = Buf("TMP32")
        XG = sb("XG", [64, 128], F32);               bXG = Buf("XG")
        XG2 = sb("XG2", [64, 128], F32);             bXG2 = Buf("XG2")
        GK = sb("GK", [64, 128], BF16);              bGK = Buf("GK")
        CB = sb("CB", [64, 2], F32);                 bCB = Buf("CB")
        PT = [sb("PT%d" % i, [128, 512], BF16) for i in range(3)]
        bPT = [Buf("PT%d" % i) for i in range(3)]
        pt_i = [0]
        OS = [sb("OS%d" % i, [128, 512], F32) for i in range(2)]
        bOS = [Buf("OS0"), Buf("OS1")]
        os_i = [0]
        XT = [sb("XT%d" % i, [128, D], F32) for i in range(2)]
        bXT = [Buf("XT0"), Buf("XT1")]
        xt_i = [0]
        XN = sb("XN", [128, D], BF16);               bXN = Buf("XN")
        JUNK = sb("JUNK", [128, D], BF16);           bJUNK = Buf("JUNK")
        YT = sb("YT", [128, 8, 128], BF16);          bYT = Buf("YT")
        OT = sb("OT", [128, D], F32);                bOT = Buf("OT")
        GG = sb("GG", [128, D], F32);                bGG = Buf("GG")
        DG = sb("DG", [128, 128], F32);              bDG = Buf("DG")
        TZ = sb("TZ", [128, 256], F32);              bTZ = Buf("TZ")
        SS = sb("SS", [128, NT], F32);               bSS = Buf("SS")
        RSTD = sb("RSTD", [128, NT], F32);           bRSTD = Buf("RSTD")
        SS2 = sb("SS2", [128, 4], F32);              bSS2 = Buf("SS2")
        RS = sb("RS", [128, 8, 1], F32);             bRS = Buf("RS")
        AC = sb("AC", [128, 4, 1], F32);             bAC = Buf("AC")
        TMPF = sb("TMPF", [128, 256], F32);          bTMPF = Buf("TMPF")
        CST = sb("CST", [128, 8, NB], F32);          bCST = Buf("CST")
        CSA = sb("CSA", [128, 8, NB], F32)
        MODT = sb("MODT", [128, L, 24, NB], F32);    bMOD = Buf("MOD")
        BADA = sb("BADA", [128, L, 24], F32)
        GPRE = sb("GPRE", [128, L, 8], F32)
        GPOST = sb("GPOST", [128, L, 8], F32)
        AMOD = sb("AMOD", [128, L, NB, 8], F32)
        GGT = sb("GGT", [128, L, NB, 8], F32)
        SC = [ps("SC%d" % i, [128, 512], F32) for i in range(3)]
        bSC = [Buf("SC%d" % i, True) for i in range(3)]
        sc_i = [0]
        OAB = [ps("OA%d" % i, [128, 512], F32) for i in range(2)]
        bOAB = [Buf("OA0", True), Buf("OA1", True)]
        oa_i = [0]
        PJ = [ps("PJ%d" % i, [128, 512], F32) for i in range(2)]
        bPJ = [Buf("PJ0", True), Buf("PJ1", True)]
        pj_i = [0]
        TP = ps("TP", [128, 1024], BF16);            bTP = Buf("TP", True)

        bYD = [[Buf("YD") for _ in range(NT)] for _ in range(NB)]

        def nxt(lst, bl, ctr):
            i = ctr[0] % len(lst)
            ctr[0] += 1
            return lst[i], bl[i]

        for t_, k in ((ident, "ident"), (identf, "identf"), (onesf, "onesf"), (TRI, "tri"), (TRIC4, "tric4"),
                      (TRIA4, "tria4"), (SH, "sh"), (TC, "tc"), (CTAB, "ctab"), (CPAD, "cpad")):
            dma(t_[:], cd[k], writes=[bC])
        op("pool", lambda: nc.gpsimd.memset(NEGH[:], -0.5), writes=[bC])
        dma(BADA[:], badaT_d, writes=[bC])
        dma(GPRE[:], gpreT_d, writes=[bC])
        dma(GPOST[:], gpostT_d, writes=[bC])
        for t_, b_ in ((QA, bQA), (KA, [bKA]), (KAs, [bKAs]), (KAw, [bKAw]), (KCA, [bKCA]), (QAn, [bQAn])):
            op("pool", lambda t_=t_: nc.gpsimd.memset(t_[:], 0.0), writes=b_)
        op("pool", lambda: nc.gpsimd.memset(MB[:], 0.0), writes=bMB)
        op("pool", lambda: nc.gpsimd.memset(VA[:], 2.0), writes=[bVA])
        op("pool", lambda: nc.gpsimd.memset(VSW[:], 1.0), writes=[bVSW])
        op("pool", lambda: nc.gpsimd.memset(VCA[:], 0.0), writes=[bVCA])
        dma(KA[64:72, :], cd["ind8"], writes=[bKA])
        dma(KA[96:100, :], cd["kcon"], writes=[bKA])
        dma(KAs[64:96, :], cd["ind32"], writes=[bKAs])
        dma(KAs[96:100, :], cd["kcon"], writes=[bKAs])
        dma(KAw[96:100, :], cd["kcon"], writes=[bKAw])
        dma(KCA[96:100, 0:127], cd["kccon"], writes=[bKCA])
        dma(VCA[0:127, 64:97], cd["vcc"], writes=[bVCA])

        def load_cast(dst_ap_fn, src_ap, ncols, bdst, nrow_p=128):
            wst, bw = nxt(WST, bWST, wst_i)
            dma(wst[0:nrow_p, :, 0:ncols], src_ap, writes=[bw])
            op("pool", lambda: nc.gpsimd.tensor_copy(out=dst_ap_fn(), in_=wst[0:nrow_p, :, 0:ncols]),
               reads=[bw], writes=[bdst])

        def silu_parts(out_ap, z_ps_ap, bps, bout, tz_ap):
            op("act", lambda: nc.scalar.activation(out=tz_ap, in_=z_ps_ap, func=AF.Tanh, scale=0.5),
               reads=[bps], writes=[bTZ])
            op("dve", lambda: nc.vector.scalar_tensor_tensor(out=out_ap, in0=tz_ap, scalar=1.0, in1=z_ps_ap,
                                                              op0=ALU.add, op1=ALU.mult),
               reads=[bTZ, bps], writes=[bout])

        def mm(out, lhsT, rhs, start, stop, reads, writes, skip=False):
            op("pe", lambda: nc.tensor.matmul(out, lhsT, rhs, start=start, stop=stop, skip_group_check=skip),
               reads=reads, writes=writes)

        dma(CST[:], cT_d, writes=[bCST])
        op("act", lambda: nc.scalar.activation(out=CSA[:], in_=CST[:], func=AF.Tanh, scale=0.5),
           reads=[bCST], writes=[bMOD])
        op("dve", lambda: nc.vector.scalar_tensor_tensor(out=CSA[:], in0=CSA[:], scalar=1.0, in1=CST[:],
                                                          op0=ALU.add, op1=ALU.mult), reads=[bMOD, bCST], writes=[bMOD])
        op("dve", lambda: nc.vector.tensor_scalar(out=CSA[:], in0=CSA[:], scalar1=0.5, scalar2=None, op0=ALU.mult),
           reads=[bMOD], writes=[bMOD])
        bCSA = bMOD
        for l in range(L):
            for blk in range(12):
                wst, bw = nxt(WST, bWST, wst_i)
                dma(wst[:, :, :], wada_d[l, :, blk * 256:(blk + 1) * 256].rearrange("(kc p) c -> p kc c", p=128),
                    writes=[bw])
                pj, bpj = nxt(PJ, bPJ, pj_i)
                for half in range(2):
                    for kc in range(8):
                        mm(pj[:, half * NB:(half + 1) * NB], wst[:, kc, half * 128:(half + 1) * 128], CSA[:, kc, :],
                           start=(kc == 0), stop=(kc == 7), reads=[bw, bCSA], writes=[bpj],
                           skip=True)
                for half in range(2):
                    j = 2 * blk + half
                    op("dve", lambda half=half, j=j, pj=pj: nc.vector.tensor_scalar(
                        out=MODT[:, l, j, :], in0=pj[:, half * NB:(half + 1) * NB], scalar1=BADA[:, l, j:j + 1],
                        scalar2=None, op0=ALU.add), reads=[bpj, bC], writes=[bMOD])
            for b in range(NB):
                op("dve", lambda b=b, l=l: nc.vector.scalar_tensor_tensor(
                    out=AMOD[:, l, b, :], in0=MODT[:, l, 8:16, b], scalar=1.0, in1=GPRE[:, l, :],
                    op0=ALU.add, op1=ALU.mult), reads=[bMOD, bC], writes=[bMOD])
                op("dve", lambda b=b, l=l: nc.vector.tensor_tensor(
                    out=GGT[:, l, b, :], in0=MODT[:, l, 16:24, b], in1=GPOST[:, l, :], op=ALU.mult),
                    reads=[bMOD, bC], writes=[bMOD])

        def prenorm(b, l, src_d):
            for tt in range(NT):
                xt, bx = nxt(XT, bXT, xt_i)
                dma(xt[:], src_d[b, tt * 128:(tt + 1) * 128, :], reads=[bYD[b][tt]], writes=[bx])
                op("act", lambda xt=xt, tt=tt: nc.scalar.activation(out=JUNK[:], in_=xt[:], func=AF.Square,
                                                                     accum_out=SS[:, tt:tt + 1]),
                   reads=[bx], writes=[bJUNK, bSS], attach=False)
                op("dve", lambda tt=tt: nc.vector.tensor_scalar(out=SS[:, tt:tt + 1], in0=SS[:, tt:tt + 1],
                                                                 scalar1=1.0 / D, scalar2=EPS, op0=ALU.mult, op1=ALU.add),
                   reads=[bSS], writes=[bSS])
                op("pool", lambda tt=tt: nc.gpsimd.tensor_tensor(out=RSTD[:, tt:tt + 1], in0=SS[:, tt:tt + 1],
                                                                  in1=NEGH[:, 0:1], op=ALU.pow),
                   reads=[bSS, bC], writes=[bRSTD])
                op("act", lambda xt=xt, tt=tt: nc.scalar.activation(out=XN[:], in_=xt[:], func=AF.Identity,
                                                                     scale=RSTD[:, tt:tt + 1]),
                   reads=[bx, bRSTD], writes=[bXN])
                for kc in range(8):
                    op("pe", lambda kc=kc: nc.tensor.transpose(out=TP[:, kc * 128:(kc + 1) * 128],
                                                               in_=XN[:, kc * 128:(kc + 1) * 128], identity=ident[:]),
                       reads=[bXN, bC], writes=[bTP])
                for kc in range(8):
                    e = "dve" if kc % 2 == 0 else "act"
                    if e == "dve":
                        op("dve", lambda kc=kc, tt=tt: nc.vector.tensor_scalar(
                            out=HT[:, kc, tt * 128:(tt + 1) * 128], in0=TP[:, kc * 128:(kc + 1) * 128],
                            scalar1=AMOD[:, l, b, kc:kc + 1], scalar2=MODT[:, l, kc, b:b + 1],
                            op0=ALU.mult, op1=ALU.add), reads=[bTP, bMOD], writes=[bHT[tt // 4]])
                    else:
                        op("act", lambda kc=kc, tt=tt: nc.scalar.activation(
                            out=HT[:, kc, tt * 128:(tt + 1) * 128], in_=TP[:, kc * 128:(kc + 1) * 128],
                            func=AF.Identity, scale=AMOD[:, l, b, kc:kc + 1], bias=MODT[:, l, kc, b:b + 1]),
                            reads=[bTP, bMOD], writes=[bHT[tt // 4]])

        def proj_fm(dst_fn, wb, c0, scale, bw, bdst_fn, sview=None):
            for c in range(4):
                pj, bpj = nxt(PJ, bPJ, pj_i)
                for kc in range(8):
                    mm(pj[0:64, :], wb[:, kc, c0:c0 + 64], HT[:, kc, c * 512:(c + 1) * 512],
                       start=(kc == 0), stop=(kc == 7), reads=[bw, bHT[c]], writes=[bpj])
                e = "act" if c % 2 == 0 else "dve"
                src = pj[0:64, :] if sview is None else sview(pj[0:64, :])
                if e == "act":
                    op("act", lambda c=c, src=src: nc.scalar.mul(out=dst_fn(c), in_=src, mul=scale),
                       reads=[bpj], writes=bdst_fn(c))
                else:
                    op("dve", lambda c=c, src=src: nc.vector.tensor_scalar(out=dst_fn(c), in0=src, scalar1=scale,
                                                                            scalar2=None, op0=ALU.mult),
                       reads=[bpj], writes=bdst_fn(c))

        def exp_tile(sc, bsc, rows, c0, c1):
            pt, bpt = nxt(PT, bPT, pt_i)
            op("act", lambda: nc.scalar.activation(out=pt[0:rows, c0:c1], in_=sc[0:rows, c0:c1], func=AF.Exp),
               reads=[bsc], writes=[bpt])
            return pt, bpt

        def acc_evac(acc, bacc, nrow):
            os_, bos = nxt(OS, bOS, os_i)
            op("dve", lambda: nc.vector.tensor_copy(out=os_[0:nrow, :], in_=acc[0:nrow, :]), reads=[bacc], writes=[bos])
            return os_, bos

        def acc_transpose(os_, bos, nrow, oav, boa):
            for j in range(4):
                op("pe", lambda j=j: nc.tensor.transpose(out=oav[:, j, :], in_=os_[0:nrow, j * 128:(j + 1) * 128],
                                                         identity=identf[0:nrow, 0:nrow]),
                   reads=[bos, bC], writes=[boa])

        def run_tiles(tiles, look=2, defer=2):
            q = []
            pend = []

            def tick(flush=False):
                while pend and (flush or pend[0][0] <= 0):
                    pend.pop(0)[1]()
                for p_ in pend:
                    p_[0] -= 1

            def emit_s(t):
                sc, bsc = nxt(SC, bSC, sc_i)
                n = len(t["s"])
                for k, (lhsT, rhs, rds) in enumerate(t["s"]):
                    mm(sc[0:t["rows"], t["c0"]:t["c1"]], lhsT, rhs, start=(k == 0), stop=(k == n - 1), reads=rds,
                       writes=[bsc])
                return sc, bsc

            def emit_rest(t, sc, bsc):
                pt, bpt = exp_tile(sc, bsc, t["rows"], t["c0"], t["c1"])
                (acc, lhsT_v, st, sp, rds, bacc) = t["pv"]
                mm(acc, lhsT_v, pt[0:t["rows"], t["c0"]:t["c1"]], start=st, stop=sp, reads=[bpt] + rds, writes=[bacc])
                tick()
                if t.get("fin") is not None:
                    nrow, accf, oav, boa, efn = t["fin"]
                    tick(flush=True)
                    os_, bos = acc_evac(accf, bacc, nrow)

                    def part2(os_=os_, bos=bos, nrow=nrow, oav=oav, boa=boa, efn=efn):
                        acc_transpose(os_, bos, nrow, oav, boa)
                        efn()
                    pend.append([defer, part2])

            for t in tiles:
                sc, bsc = emit_s(t)
                q.append((t, sc, bsc))
                if len(q) > look:
                    emit_rest(*q.pop(0))
            while q:
                emit_rest(*q.pop(0))
            tick(flush=True)

        def moba_head(b, l, h):
            col0 = 256 * h
            load_cast(lambda: WBm[:, :, :], win_d[l, :, col0:col0 + 256].rearrange("(kc p) c -> p kc c", p=128),
                      256, bWBm)
            dma(QA[96:100, :], cd["qcm"][h], writes=bQA)
            proj_fm(lambda c: QA[0:64, c * 512:(c + 1) * 512], WBm, 0, 0.125, bWBm, lambda c: [bQA[c]])
            proj_fm(lambda c: KA[0:64, c * 512:(c + 1) * 512], WBm, 64, 1.0, bWBm, lambda c: [bKA])
            for g4 in range(4):
                pj, bpj = nxt(PJ, bPJ, pj_i)
                for t4 in range(4):
                    tt = g4 * 4 + t4
                    for kc in range(8):
                        mm(pj[:, t4 * 128:(t4 + 1) * 128], HT[:, kc, tt * 128:(tt + 1) * 128], WBm[:, kc, 128:256],
                           start=(kc == 0), stop=(kc == 7), reads=[bHT[g4], bWBm], writes=[bpj], skip=True)
                pjv = pj[:, :].rearrange("p (t c) -> p t c", c=128)
                op("dve", lambda pjv=pjv, g4=g4: nc.vector.tensor_copy(out=VA[:, g4 * 4:(g4 + 1) * 4, 0:64],
                                                                       in_=pjv[:, :, 0:64]),
                   reads=[bpj], writes=[bVA])
                tzv = TZ[:, 0:256].rearrange("p (t c) -> p t c", c=64)
                silu_parts(SZ[:, g4 * 4:(g4 + 1) * 4, :], pjv[:, :, 64:128], bpj, bSZ, tzv)
            op("dve", lambda: nc.vector.tensor_reduce(out=KMf[:, :], in_=KA[0:64, :].rearrange("p (n k) -> p n k", k=256),
                                                      axis=AX.X, op=ALU.add), reads=[bKA], writes=[bKM])
            op("dve", lambda: nc.vector.tensor_scalar(out=KMb[:, :], in0=KMf[:, :], scalar1=1.0 / 256, scalar2=None,
                                                      op0=ALU.mult), reads=[bKM], writes=[bKM])
            pj, bpj = nxt(PJ, bPJ, pj_i)
            for tt in range(NT):
                mm(pj[:, tt * 8:(tt + 1) * 8], QA[0:64, tt * 128:(tt + 1) * 128], KMb[:, :], start=True, stop=True,
                   reads=[bQA[tt // 4], bKM], writes=[bpj], skip=True)
            op("dve", lambda pj=pj: nc.vector.tensor_tensor(out=GP[:], in0=pj[:, 0:128].rearrange("p (t n) -> p t n", n=8),
                                                            in1=CPAD[:], op=ALU.add), reads=[bpj, bC], writes=[bGP])
            op("dve", lambda: nc.vector.tensor_scalar(out=MB[:, 0:6, 64:72], in0=GP[:, 0:6, :], scalar1=-1e8, scalar2=NEGM,
                                                      op0=ALU.is_lt, op1=ALU.mult), reads=[bGP], writes=[bMB[0], bMB[1]])
            for tt in range(6, NT):
                op("dve", lambda tt=tt: nc.vector.max(out=M8[:, :], in_=GP[:, tt, :]), reads=[bGP], writes=[bM8])
                op("dve", lambda tt=tt: nc.vector.tensor_scalar(out=MB[:, tt, 64:72], in0=GP[:, tt, :], scalar1=M8[:, 3:4],
                                                                 scalar2=NEGM, op0=ALU.is_lt, op1=ALU.mult),
                   reads=[bGP, bM8], writes=[bMB[tt // 4]])
            for c in range(4):
                for t4 in range(4):
                    tt = 4 * c + t4
                    op("pe", lambda tt=tt, t4=t4: nc.tensor.transpose(out=TP[0:96, t4 * 128:(t4 + 1) * 128],
                                                                      in_=MB[:, tt, :], identity=ident[:]),
                       reads=[bMB[c], bC], writes=[bTP])
                op("act", lambda c=c: nc.scalar.copy(out=QA[64:72, c * 512:(c + 1) * 512], in_=TP[64:72, 0:512]),
                   reads=[bTP], writes=[bQA[c]])
            tiles = []
            for c in range(4):
                oa, boa = nxt(OAB, bOAB, oa_i)
                oav = oa[:, 0:260].rearrange("p (j c) -> p j c", c=65)
                nkt = 4 * c + 4

                acc, bacc = nxt(PJ, bPJ, pj_i)

                def epi(c=c, oav=oav, boa=boa):
                    op("dve", lambda: nc.vector.reciprocal(out=RS[:, 0:4, :], in_=oav[:, :, 64:65]),
                       reads=[boa], writes=[bRS])
                    for j2 in range(4):
                        tt = 4 * c + j2
                        op("dve", lambda j2=j2, tt=tt: nc.vector.scalar_tensor_tensor(
                            out=Y[:, tt, h * 64:(h + 1) * 64], in0=oav[:, j2, 0:64], scalar=RS[:, j2, :],
                            in1=SZ[:, tt, :], op0=ALU.mult, op1=ALU.mult), reads=[boa, bRS, bSZ], writes=[bY[tt]])

                for kt in range(nkt):
                    j0 = max(0, kt - 4 * c)
                    c0 = 128 * j0
                    diag = kt >= 4 * c
                    smm = [(KA[0:100, kt * 128:(kt + 1) * 128], QA[0:100, c * 512 + c0:(c + 1) * 512], [bKA, bQA[c]])]
                    if diag:
                        smm.append((ident[:, :], TRI[:, 0:512 - c0], [bC]))
                    pv = (acc[0:65, c0:512], VA[:, kt, :], (kt == 0), (kt == nkt - 1), [bVA], bacc)
                    tiles.append(dict(rows=128, c0=c0, c1=512, s=smm, pv=pv,
                                      fin=(65, acc, oav, boa, epi) if kt == nkt - 1 else None))
            run_tiles(tiles)

        def gelu_fm(src_ps, bsrc, bias_ap, out_bf):
            op("act", lambda: nc.scalar.activation(out=XG[:, 0:127], in_=src_ps, func=AF.Identity, bias=bias_ap),
               reads=[bsrc, bCB], writes=[bXG])
            op("dve", lambda: nc.vector.tensor_tensor(out=XG2[:, 0:127], in0=XG[:, 0:127], in1=XG[:, 0:127], op=ALU.mult),
               reads=[bXG], writes=[bXG2])
            op("dve", lambda: nc.vector.tensor_scalar(out=XG2[:, 0:127], in0=XG2[:, 0:127], scalar1=0.044715, scalar2=1.0,
                                                      op0=ALU.mult, op1=ALU.add), reads=[bXG2], writes=[bXG2])
            op("dve", lambda: nc.vector.tensor_tensor(out=XG2[:, 0:127], in0=XG2[:, 0:127], in1=XG[:, 0:127], op=ALU.mult),
               reads=[bXG2, bXG], writes=[bXG2])
            op("act", lambda: nc.scalar.activation(out=XG2[:, 0:127], in_=XG2[:, 0:127], func=AF.Tanh,
                                                   scale=0.7978845608028654), reads=[bXG2], writes=[bXG2])
            op("dve", lambda: nc.vector.scalar_tensor_tensor(out=XG2[:, 0:127], in0=XG2[:, 0:127], scalar=1.0,
                                                              in1=XG[:, 0:127], op0=ALU.add, op1=ALU.mult),
               reads=[bXG2, bXG], writes=[bXG2])
            op("dve", lambda: nc.vector.tensor_scalar(out=out_bf, in0=XG2[:, 0:127], scalar1=0.5, scalar2=None,
                                                      op0=ALU.mult), reads=[bXG2], writes=[bGK])

        def nsa_group(b, l, g):
            col0 = 2048 + 908 * g
            for (a0, a1) in ((0, 256), (256, 512)):
                load_cast(lambda a0=a0, a1=a1: WBn[:, :, a0:a1],
                          win_d[l, :, col0 + a0:col0 + a1].rearrange("(kc p) c -> p kc c", p=128), a1 - a0, bWBn)
            QAv = QAn[:, :].rearrange("p (t j i) -> p t j i", j=4, i=128)

            def compress():
                for kv, wc0 in (("k", 256), ("v", 320)):
                    proj_fm(lambda c, kv=kv: KCR[kv][:, c * 512:(c + 1) * 512], WBn, wc0, 1.0, bWBn,
                            lambda c, kv=kv: [bKCR[kv]])
                    kcr = KCR[kv][:, :].rearrange("p (t r) -> p t r", r=16)
                    pj, bpj = nxt(PJ, bPJ, pj_i)
                    for l_ in range(32):
                        mm(pj[0:64, 200:201], WC1[kv][:, l_, :], PEb[kv][:, l_:l_ + 1], start=(l_ == 0), stop=(l_ == 31),
                           reads=[bWC], writes=[bpj], skip=True)
                    ci = 0 if kv == "k" else 1
                    op("dve", lambda pj=pj, ci=ci: nc.vector.tensor_copy(out=CB[:, ci:ci + 1], in_=pj[0:64, 200:201]),
                       reads=[bpj], writes=[bCB])
                    pj2, bpj2 = nxt(PJ, bPJ, pj_i)
                    for l_ in range(32):
                        rhs = kcr[:, 0:127, l_] if l_ < 16 else kcr[:, 1:128, l_ - 16]
                        mm(pj2[0:64, 0:127], WC1[kv][:, l_, :], rhs, start=(l_ == 0), stop=(l_ == 31),
                           reads=[bWC, bKCR[kv]], writes=[bpj2])
                    gelu_fm(pj2[0:64, 0:127], bpj2, CB[:, ci:ci + 1], GK[:, 0:127])
                    pj3, bpj3 = nxt(PJ, bPJ, pj_i)
                    if kv == "k":
                        mm(pj3[0:64, 0:127], W2["k"][:, :], GK[:, 0:127], start=True, stop=True, reads=[bWC, bGK],
                           writes=[bpj3])
                        op("dve", lambda pj3=pj3: nc.vector.tensor_copy(out=KCA[0:64, 0:127], in_=pj3[0:64, 0:127]),
                           reads=[bpj3], writes=[bKCA])
                    else:
                        mm(pj3[0:127, 0:64], GK[:, 0:127], W2["v"][:, :], start=True, stop=True, reads=[bWC, bGK],
                           writes=[bpj3])
                        op("dve", lambda pj3=pj3: nc.vector.tensor_copy(out=VCA[0:127, 0:64], in_=pj3[0:127, 0:64]),
                           reads=[bpj3], writes=[bVCA])


            dma(QAn[96:100, :], cd["qcn"][g], writes=[bQAn])
            for j in range(4):
                proj_fm(lambda c, j=j: QAv[0:64, 4 * c:4 * c + 4, j, :], WBn, 64 * j, 0.125, bWBn, lambda c: [bQAn],
                        sview=lambda a: a.rearrange("p (t i) -> p t i", i=128))
            proj_fm(lambda c: KAs[0:64, c * 512:(c + 1) * 512], WBn, 384, 1.0, bWBn, lambda c: [bKAs])
            proj_fm(lambda c: KAw[0:64, c * 512:(c + 1) * 512], WBn, 448, 1.0, bWBn, lambda c: [bKAw])
            if SUB < 1:
                return
            compress()
            if SUB < 2:
                return
            for (a0, a1) in ((512, 768), (768, 908)):
                load_cast(lambda a0=a0, a1=a1: WBn[:, :, a0 - 512:a1 - 512],
                          win_d[l, :, col0 + a0:col0 + a1].rearrange("(kc p) c -> p kc c", p=128), a1 - a0, bWBn)
            for tt in range(NT):
                pj, bpj = nxt(PJ, bPJ, pj_i)
                for kc in range(8):
                    mm(pj[:, 0:396], HT[:, kc, tt * 128:(tt + 1) * 128], WBn[:, kc, 0:396],
                       start=(kc == 0), stop=(kc == 7), reads=[bHT[tt // 4], bWBn], writes=[bpj])
                op("dve", lambda pj=pj, tt=tt: nc.vector.tensor_copy(
                    out=VSW[:, tt, :, 0:64], in_=pj[:, 0:128].rearrange("p (a c) -> p a c", c=64)),
                    reads=[bpj], writes=[bVSW])
                op("act", lambda pj=pj, tt=tt: nc.scalar.activation(out=GT[:, tt, :], in_=pj[:, 128:140], func=AF.Tanh,
                                                                     scale=0.5), reads=[bpj], writes=[bGT])
                silu_parts(SZn[:, tt, :], pj[:, 140:396], bpj, bSZn, TZ[:, 0:256])
            op("dve", lambda: nc.vector.tensor_scalar(out=GT[:], in0=GT[:], scalar1=0.5, scalar2=0.5, op0=ALU.mult,
                                                      op1=ALU.add), reads=[bGT], writes=[bGT])
            def branch_epilogue(oav, boa, tt, gi, first, last):
                op("dve", lambda: nc.vector.tensor_scalar(out=RS[:, 0:4, :], in0=oav[:, :, 64:65], scalar1=1e-30,
                                                          scalar2=None, op0=ALU.add), reads=[boa], writes=[bRS])
                op("dve", lambda: nc.vector.reciprocal(out=RS[:, 0:4, :], in_=RS[:, 0:4, :]), reads=[bRS], writes=[bRS])
                gv = GT[:, tt, :].rearrange("p (j r) -> p j r", r=3)
                op("dve", lambda: nc.vector.tensor_tensor(out=AC[:, :, :], in0=RS[:, 0:4, :], in1=gv[:, :, gi:gi + 1],
                                                          op=ALU.mult), reads=[bRS, bGT], writes=[bAC])
                ocv = OC[:, tt, :].rearrange("p (j c) -> p j c", c=64)
                for j in range(4):
                    if first:
                        op("dve", lambda j=j: nc.vector.tensor_scalar(out=ocv[:, j, :], in0=oav[:, j, 0:64],
                                                                      scalar1=AC[:, j, :], scalar2=None, op0=ALU.mult),
                           reads=[boa, bAC], writes=[bOC[tt]])
                    else:
                        op("dve", lambda j=j: nc.vector.scalar_tensor_tensor(
                            out=ocv[:, j, :], in0=oav[:, j, 0:64], scalar=AC[:, j, :], in1=ocv[:, j, :],
                            op0=ALU.mult, op1=ALU.add), reads=[boa, bAC, bOC[tt]], writes=[bOC[tt]])
                if last:
                    yc = 512 + 256 * g
                    op("dve", lambda: nc.vector.scalar_tensor_tensor(
                        out=Y[:, tt, yc:yc + 256], in0=OC[:, tt, :], scalar=0.5, in1=SZn[:, tt, :],
                        op0=ALU.mult, op1=ALU.mult), reads=[bOC[tt], bSZn], writes=[bY[tt]])

            if SUB < 3:
                return
            tiles = []
            for tt in range(NT):
                oa, boa = nxt(OAB, bOAB, oa_i)
                oav = oa[:, 0:388].rearrange("p (j c) -> p j c", c=97)

                acc, bacc = nxt(PJ, bPJ, pj_i)

                def epiA(tt=tt, oav=oav, boa=boa):
                    branch_epilogue(oav, boa, tt, 0, True, False)
                    for j in range(4):
                        in1 = CTAB[:, tt, :] if j == 0 else IMP[:, tt, :]
                        op("dve", lambda j=j, in1=in1: nc.vector.scalar_tensor_tensor(
                            out=IMP[:, tt, :], in0=oav[:, j, 65:97], scalar=RS[:, j, :], in1=in1,
                            op0=ALU.mult, op1=ALU.add), reads=[boa, bRS, bC, bIMP[tt]], writes=[bIMP[tt]])
                    if tt >= 8:
                        op("dve", lambda: nc.vector.max(out=M8[:, :], in_=IMP[:, tt, :]), reads=[bIMP[tt]], writes=[bM8])
                        op("dve", lambda: nc.vector.match_replace(out=TMP32[:, :], in_to_replace=M8[:, :],
                                                                  in_values=IMP[:, tt, :], imm_value=-3e38),
                           reads=[bM8, bIMP[tt]], writes=[bTMP32], attach=False)
                        op("dve", lambda: nc.vector.max(out=M8b[:, :], in_=TMP32[:, :]), reads=[bTMP32], writes=[bM8b])
                        op("dve", lambda: nc.vector.tensor_scalar(out=MB[:, tt, 64:96], in0=IMP[:, tt, :],
                                                                  scalar1=M8b[:, 7:8], scalar2=NEGM, op0=ALU.is_lt,
                                                                  op1=ALU.mult), reads=[bIMP[tt], bM8b],
                           writes=[bMB[tt // 4]])
                    else:
                        op("dve", lambda: nc.vector.tensor_scalar(out=MB[:, tt, 64:96], in0=IMP[:, tt, :], scalar1=-1e8,
                                                                  scalar2=NEGM, op0=ALU.is_lt, op1=ALU.mult),
                           reads=[bIMP[tt]], writes=[bMB[tt // 4]])

                smm = [(KCA[0:100, 0:127], QAn[0:100, tt * 512:(tt + 1) * 512], [bKCA, bQAn]),
                       (SH[:, 128 - 8 * tt:128 - 8 * tt + 127], TC[:, :], [bC])]
                pv = (acc[0:97, 0:512], VCA[0:127, :], True, True, [bVCA], bacc)
                tiles.append(dict(rows=127, c0=0, c1=512, s=smm, pv=pv, fin=(97, acc, oav, boa, epiA)))
            if SUB < 4:
                run_tiles(tiles)
                return
            for tt in range(NT):
                oa, boa = nxt(OAB, bOAB, oa_i)
                oav = oa[:, 0:260].rearrange("p (j c) -> p j c", c=65)
                kts = [kt for kt in range(tt - 4, tt + 1) if kt >= 0]

                acc, bacc = nxt(PJ, bPJ, pj_i)

                def epiW(tt=tt, oav=oav, boa=boa):
                    branch_epilogue(oav, boa, tt, 2, False, False)

                for kt in kts:
                    extra = TRIC4 if kt == tt else (TRIA4 if kt == tt - 4 else None)
                    smm = [(KAw[0:100, kt * 128:(kt + 1) * 128], QAn[0:100, tt * 512:(tt + 1) * 512], [bKAw, bQAn])]
                    if extra is not None:
                        smm.append((ident[:, :], extra[:, :], [bC]))
                    pv = (acc[0:65, 0:512], VSW[:, kt, 1, :], (kt == kts[0]), (kt == kts[-1]), [bVSW], bacc)
                    tiles.append(dict(rows=128, c0=0, c1=512, s=smm, pv=pv,
                                      fin=(65, acc, oav, boa, epiW) if kt == kts[-1] else None))
            run_tiles(tiles)
            if SUB < 5:
                return
            for c in range(4):
                for t4 in range(4):
                    tt = 4 * c + t4
                    op("pe", lambda tt=tt, t4=t4: nc.tensor.transpose(out=TP[0:96, t4 * 128:(t4 + 1) * 128],
                                                                      in_=MB[:, tt, :], identity=ident[:]),
                       reads=[bMB[c], bC], writes=[bTP])
                tpv = TP[64:96, 0:512].rearrange("p (t i) -> p t i", i=128)
                if TVAR == 1:
                    continue
                for j in range(4):
                    e = "act" if (j % 2 == 0 or TVAR == 2) else "dve"
                    if e == "act":
                        op("act", lambda j=j, c=c, tpv=tpv: nc.scalar.copy(out=QAv[64:96, 4 * c:4 * c + 4, j, :], in_=tpv),
                           reads=[bTP], writes=[bQAn])
                    else:
                        op("dve", lambda j=j, c=c, tpv=tpv: nc.vector.tensor_copy(out=QAv[64:96, 4 * c:4 * c + 4, j, :],
                                                                                  in_=tpv), reads=[bTP], writes=[bQAn])
            if SUB < 6:
                return
            tiles = []
            for tt in range(NT):
                oa, boa = nxt(OAB, bOAB, oa_i)
                oav = oa[:, 0:260].rearrange("p (j c) -> p j c", c=65)

                acc, bacc = nxt(PJ, bPJ, pj_i)

                def epiS(tt=tt, oav=oav, boa=boa):
                    branch_epilogue(oav, boa, tt, 1, False, True)

                for kt in range(tt + 1):
                    smm = [(KAs[0:100, kt * 128:(kt + 1) * 128], QAn[0:100, tt * 512:(tt + 1) * 512], [bKAs, bQAn])]
                    if kt == tt:
                        smm.append((ident[:, :], TRIC4[:, :], [bC]))
                    pv = (acc[0:65, 0:512], VSW[:, kt, 0, :], (kt == 0), (kt == tt), [bVSW], bacc)
                    tiles.append(dict(rows=128, c0=0, c1=512, s=smm, pv=pv,
                                      fin=(65, acc, oav, boa, epiS) if kt == tt else None))
            run_tiles(tiles)

        def outproj(b, l, src_d):
            WO = QAn[:, :].rearrange("p (kc c) -> p kc c", c=1024)
            for blk in range(4):
                load_cast(lambda blk=blk: WO[:, :, blk * 256:(blk + 1) * 256],
                          wout_d[l, :, blk * 256:(blk + 1) * 256].rearrange("(kc p) c -> p kc c", p=128), 256, bQAn)
            for kc in range(8):
                op("dve", lambda kc=kc: nc.vector.tensor_scalar(out=DG[:, :], in0=identf[:, :],
                                                                scalar1=GGT[:, l, b, kc:kc + 1], scalar2=None,
                                                                op0=ALU.mult), reads=[bC, bMOD], writes=[bDG])
                pj, bpj = nxt(PJ, bPJ, pj_i)
                mm(pj[:, 0:128], onesf[:, :], DG[:, :], start=True, stop=True, reads=[bC, bDG], writes=[bpj])
                op("act", lambda kc=kc, pj=pj: nc.scalar.copy(out=GG[:, kc * 128:(kc + 1) * 128], in_=pj[:, 0:128]),
                   reads=[bpj], writes=[bGG])
            for tt in range(NT):
                for kc in range(8):
                    op("pe", lambda kc=kc, tt=tt: nc.tensor.transpose(out=TP[:, kc * 128:(kc + 1) * 128],
                                                                      in_=Y[:, tt, kc * 128:(kc + 1) * 128],
                                                                      identity=ident[:]),
                       reads=[bY[tt], bC], writes=[bTP])
                op("act", lambda: nc.scalar.copy(out=YT[:, 0:4, :], in_=TP[:, 0:512].rearrange("p (k i) -> p k i", i=128)),
                   reads=[bTP], writes=[bYT])
                op("dve", lambda: nc.vector.tensor_copy(out=YT[:, 4:8, :],
                                                        in_=TP[:, 512:1024].rearrange("p (k i) -> p k i", i=128)),
                   reads=[bTP], writes=[bYT])
                xt, bx = nxt(XT, bXT, xt_i)
                dma(xt[:], src_d[b, tt * 128:(tt + 1) * 128, :], reads=[bYD[b][tt]], writes=[bx])
                pjs = []
                for half in range(2):
                    pj, bpj = nxt(PJ, bPJ, pj_i)
                    for kc in range(8):
                        mm(pj[:, :], YT[:, kc, :], WO[:, kc, half * 512:(half + 1) * 512], start=(kc == 0),
                           stop=(kc == 7), reads=[bYT, bQAn], writes=[bpj])
                    op("act", lambda pj=pj, half=half: nc.scalar.activation(
                        out=JUNK[:, half * 512:(half + 1) * 512], in_=pj[:, :], func=AF.Square,
                        accum_out=SS2[:, half:half + 1]), reads=[bpj], writes=[bJUNK, bSS2], attach=False)
                    pjs.append((pj, bpj))
                op("dve", lambda: nc.vector.tensor_tensor(out=SS2[:, 2:3], in0=SS2[:, 0:1], in1=SS2[:, 1:2], op=ALU.add),
                   reads=[bSS2], writes=[bSS2])
                op("dve", lambda: nc.vector.tensor_scalar(out=SS2[:, 2:3], in0=SS2[:, 2:3], scalar1=1.0 / D, scalar2=EPS,
                                                          op0=ALU.mult, op1=ALU.add), reads=[bSS2], writes=[bSS2])
                op("pool", lambda: nc.gpsimd.tensor_tensor(out=SS2[:, 3:4], in0=SS2[:, 2:3], in1=NEGH[:, 0:1], op=ALU.pow),
                   reads=[bSS2, bC], writes=[bSS2])
                for half in range(2):
                    pj, bpj = pjs[half]
                    sl = slice(half * 512, (half + 1) * 512)
                    op("dve", lambda pj=pj, sl=sl: nc.vector.scalar_tensor_tensor(
                        out=OT[:, sl], in0=pj[:, :], scalar=SS2[:, 3:4], in1=GG[:, sl], op0=ALU.mult, op1=ALU.mult),
                        reads=[bpj, bSS2, bGG], writes=[bOT])
                op("pool", lambda xt=xt: nc.gpsimd.tensor_tensor(out=OT[:, :], in0=OT[:, :], in1=xt[:, :], op=ALU.add),
                   reads=[bOT, bx], writes=[bOT])
                dma(y_d[b, tt * 128:(tt + 1) * 128, :], OT[:, :], reads=[bOT], writes=[bYD[b][tt]])

        for b in range(NB):
            for l in range(L):
                src = x_d if l == 0 else y_d
                if STAGE < 2:
                    continue
                prenorm(b, l, src)
                if STAGE < 3:
                    continue
                for kv in "kv":
                    wst, bw = nxt(WST, bWST, wst_i)
                    wv = wst[0:64, :, :].rearrange("p a c -> p (a c)")
                    dma(wv, wc1_d[kv][l], writes=[bw])
                    op("pool", lambda kv=kv, wv=wv: nc.gpsimd.tensor_copy(
                        out=WC1[kv][:, :, :].rearrange("p a c -> p (a c)"), in_=wv), reads=[bw], writes=[bWC])
                    wst, bw = nxt(WST, bWST, wst_i)
                    wv2 = wst[0:64, 0, 0:32]
                    wv3 = wst[0:64, 1, 0:64]
                    dma(wv2, peT_d[kv][l], writes=[bw])
                    dma(wv3, w2_d[kv][l], writes=[bw])
                    op("pool", lambda kv=kv, wv2=wv2: nc.gpsimd.tensor_copy(out=PEb[kv][:, :], in_=wv2), reads=[bw],
                       writes=[bWC])
                    op("pool", lambda kv=kv, wv3=wv3: nc.gpsimd.tensor_copy(out=W2[kv][:, :], in_=wv3), reads=[bw],
                       writes=[bWC])
                for h in range(8 if STAGE >= 5 else (1 if STAGE == 4 else 0)):
                    moba_head(b, l, h)
                for g in range(2 if STAGE >= 7 else (1 if STAGE == 6 else 0)):
                    nsa_group(b, l, g)
                if STAGE < 8:
                    continue
                if debug and b == 0 and l == 0:
                    for tt in range(NT):
                        dma(dbg_d[tt * 128:(tt + 1) * 128, :], Y[:, tt, :], reads=[bY[tt]])
                outproj(b, l, src)
        sy.finish()
        build.stats = (sy.nops, sy.nwaits)
    return nc


def prep_shared(inputs, L):
    f = lambda a: np.ascontiguousarray(np.asarray(a, dtype=np.float32))
    perm = in_perm()
    sh = {}
    sh["w_ada"] = f(inputs["w_ada"][:L])
    sh["b_adaT"] = f(np.asarray(inputs["b_ada"][:L]).reshape(L, 24, 128).transpose(2, 0, 1))
    sh["g_preT"] = f(np.asarray(inputs["g_pre"][:L]).reshape(L, 8, 128).transpose(2, 0, 1))
    sh["g_postT"] = f(np.asarray(inputs["g_post"][:L]).reshape(L, 8, 128).transpose(2, 0, 1))
    sh["w_in"] = f(np.asarray(inputs["w_in"][:L])[:, :, perm])
    sh["w_out"] = f(inputs["w_out"][:L])
    for kv, n1, n2, npe in (("k", "w_ck1", "w_ck2", "pe_k"), ("v", "w_cv1", "w_cv2", "pe_v")):
        w1 = np.asarray(inputs[n1][:L]).reshape(L, 32, 64, 64).transpose(0, 2, 1, 3).reshape(L, 64, 2048)
        sh["wc1" + kv] = f(w1)
        sh["peT" + kv] = f(np.asarray(inputs[npe][:L]).transpose(0, 2, 1))
        sh["w2" + kv] = f(inputs[n2][:L])
    return sh


_CACHE = {}


def run(inputs, n_cores, NB, L, debug=False):
    consts = make_consts()
    key = (NB, L, debug)
    if key not in _CACHE:
        _CACHE[key] = build(NB, L, consts, debug=debug)
    nc = _CACHE[key]
    sh = prep_shared(inputs, L)
    for k, v in consts.items():
        sh["c_" + k] = v
    x = np.asarray(inputs["x"], dtype=np.float32)
    c = np.asarray(inputs["c"], dtype=np.float32)
    in_maps = []
    for i in range(n_cores):
        m = dict(sh)
        m["x"] = np.ascontiguousarray(x[i * NB:(i + 1) * NB])
        ci = c[i * NB:(i + 1) * NB]
        m["cT"] = np.ascontiguousarray(ci.reshape(NB, 8, 128).transpose(2, 1, 0))
        in_maps.append(m)
    res = run_bass_kernel_spmd(nc, in_maps, core_ids=list(range(n_cores)))
    out = np.concatenate([np.asarray(r["y"]) for r in res.results], axis=0)
    if debug:
        return out, np.asarray(res.results[0]["dbg"])
    return out


def kernel(x, c, w_ada, b_ada, g_pre, g_post, w_in, w_out, pe_k, pe_v, w_ck1, w_ck2, w_cv1, w_cv2):
    inputs = dict(x=x, c=c, w_ada=w_ada, b_ada=b_ada, g_pre=g_pre, g_post=g_post, w_in=w_in, w_out=w_out,
                  pe_k=pe_k, pe_v=pe_v, w_ck1=w_ck1, w_ck2=w_ck2, w_cv1=w_cv1, w_cv2=w_cv2)
    NB = x.shape[0] // N_CORES
    out = run(inputs, N_CORES, NB, 2)
    return out.astype(np.float32, copy=False)
```

```python
import contextlib
import numpy as np
import ml_dtypes
import concourse.bass as bass
import concourse.mybir as mybir
from concourse.bass_utils import run_bass_kernel_spmd

F32 = mybir.dt.float32
BF16 = mybir.dt.bfloat16
AF = mybir.ActivationFunctionType
ALU = mybir.AluOpType
AX = mybir.AxisListType

D = 1024
S = 2048
NT = 16
DIN = 3864
NEGM = -30000.0
EPS = 1e-6
N_CORES = 8


class Buf:
    __slots__ = ("w", "r", "name", "excl")

    def __init__(self, name="", excl=False):
        self.w = None
        self.r = {}
        self.name = name
        self.excl = excl


class Sync:
    def __init__(self, nc, es, n_dma_sems=12):
        self.nc = nc
        self.eng = {"pe": nc.tensor, "act": nc.scalar, "dve": nc.vector, "pool": nc.gpsimd, "sp": nc.sync}
        self.sem = {}
        self.cnt = {}
        self.known = {k: {} for k in self.eng}
        for k in ("pe", "act", "dve", "pool"):
            self.sem[k] = es.enter_context(nc.semaphore("s_" + k))
            self.cnt[k] = 0
        self.dsem = []
        for i in range(n_dma_sems):
            k = "d%d" % i
            self.sem[k] = es.enter_context(nc.semaphore("s_" + k))
            self.cnt[k] = 0
            self.dsem.append(k)
        self.dnext = 0
        self.nwaits = 0
        self.nops = 0

    def _wait(self, e, tok):
        k, v = tok
        if self.known[e].get(k, 0) >= v:
            return
        self.known[e][k] = v
        self.eng[e].wait_ge(self.sem[k], v)
        self.nwaits += 1

    def _deps(self, e, reads, writes, defer=False):
        deps = []
        for b in reads:
            if b.w is not None:
                deps.append(b.w)
        for b in writes:
            if b.w is not None:
                deps.append(b.w)
            for k, v in b.r.items():
                deps.append((k, v))
        need = {}
        for k, v in deps:
            if e == "pe" and k == "pe":
                continue
            if self.known[e].get(k, 0) >= v:
                continue
            if need.get(k, 0) < v:
                need[k] = v
        items = list(need.items())
        last = None
        if defer and items:
            last = items.pop()
        for k, v in items:
            self._wait(e, (k, v))
        return last

    def _record(self, tok, reads, writes):
        for b in reads:
            if b.r.get(tok[0], 0) < tok[1]:
                b.r[tok[0]] = tok[1]
        for b in writes:
            b.w = tok
            b.r = {}

    def op(self, e, fn, reads=(), writes=(), attach=True):
        ex = [b for b in reads if b.excl]
        if ex:
            writes = list(writes) + ex
        last = self._deps(e, reads, writes, defer=attach and ATTACH_WAITS)
        ins = fn()
        if last is not None:
            k, v = last
            self.known[e][k] = v
            ins._wait_ge(self.sem[k], v)
        self.cnt[e] += 1
        ins.then_inc(self.sem[e], 1)
        tok = (e, self.cnt[e])
        self._record(tok, reads, writes)
        self.nops += 1
        return tok

    def dma(self, out, in_, reads=(), writes=(), q="sp"):
        k = self.dsem[self.dnext]
        self.dnext = (self.dnext + 1) % len(self.dsem)
        if self.cnt[k] > 0:
            self._wait(q, (k, self.cnt[k]))
        self._deps(q, reads, writes)
        ins = self.eng[q].dma_start(out=out, in_=in_)
        self.cnt[k] += 16
        ins.then_inc(self.sem[k], 16)
        tok = (k, self.cnt[k])
        self._record(tok, reads, writes)
        self.nops += 1
        return tok

    def finish(self):
        for k in self.dsem:
            if self.cnt[k] > 0:
                self._wait("sp", (k, self.cnt[k]))


def _bf(a):
    return np.ascontiguousarray(a.astype(np.float32)).astype(ml_dtypes.bfloat16)


def make_consts():
    c = {}
    p = np.arange(128)[:, None]
    m = np.arange(512)[None, :]
    c["ident"] = _bf(np.eye(128))
    c["identf"] = np.eye(128, dtype=np.float32)
    c["onesf"] = np.ones((128, 128), dtype=np.float32)
    c["tri"] = _bf(np.where((m < 128) & (p > m), NEGM, 0.0))
    i4 = m % 128
    c["tric4"] = _bf(np.where(p > i4, NEGM, 0.0))
    c["tria4"] = _bf(np.where(p <= i4, NEGM, 0.0))
    sh = np.zeros((128, 256), np.float32)
    for u in range(8):
        sh[u, u + 127] = 1.0
    sh[8, 135:] = 1.0
    c["sh"] = _bf(sh)
    tc = np.zeros((128, 512), np.float32)
    for u in range(8):
        tc[u, :] = np.where(i4[0] >= 16 * u + 15, 0.0, NEGM)
    tc[8, :] = NEGM
    c["tc"] = _bf(tc)
    t = (np.arange(NT)[None, :, None] * 128 + np.arange(128)[:, None, None])
    n = np.arange(32)[None, None, :]
    blk = t // 64
    forced = (n == 0) | (n == blk) | (n == blk - 1)
    ctab = np.where(n > blk, -1e9, np.where(forced, 1e4, 0.0)).astype(np.float32)
    c["ctab"] = np.ascontiguousarray(ctab)
    cur = (np.arange(NT) // 2)[None, :, None]
    n8 = np.arange(8)[None, None, :]
    cpad = np.where(n8 == cur, 1e9, np.where(n8 > cur, -1e9, 0.0)).astype(np.float32)
    c["cpad"] = np.ascontiguousarray(np.broadcast_to(cpad, (128, NT, 8)))
    tt = np.arange(S)
    a, b = tt // 64, tt % 64
    slopes = 2.0 ** (-np.arange(1, 9, dtype=np.float64))
    qc = np.zeros((16, 4, S), np.float32)
    for h in range(16):
        s = slopes[h % 8]
        qc[h, 0] = -64.0 * s * a
        qc[h, 1] = -s * b
        qc[h, 2] = 64.0 * s
        qc[h, 3] = s
    c["qcm"] = _bf(qc[:8])
    qn = qc[8:].reshape(2, 4, 4, NT, 128)
    c["qcn"] = _bf(qn.transpose(0, 2, 3, 1, 4).reshape(2, 4, NT * 4 * 128))
    kc = np.stack([np.ones(S), np.ones(S), a, b]).astype(np.float32)
    c["kcon"] = _bf(kc)
    pos = 16 * np.arange(127) + 31
    c["kccon"] = _bf(np.stack([np.ones(127), np.ones(127), pos // 64, pos % 64]))
    c["ind8"] = _bf((tt[None, :] // 256 == np.arange(8)[:, None]))
    c["ind32"] = _bf((tt[None, :] // 64 == np.arange(32)[:, None]))
    i = np.arange(127)[:, None]
    j = np.arange(32)[None, :]
    ov = (i * 16 < (j + 1) * 64) & (i * 16 + 32 > j * 64)
    vcc = np.zeros((127, 33), np.float32)
    vcc[:, 0] = 1.0
    vcc[:, 1:] = ov
    c["vcc"] = _bf(vcc)
    return c


CONST_SHAPES = None


def in_perm():
    offs = np.cumsum([0, 512, 512, 512, 512, 512, 128, 128, 128, 128, 128, 128, 24, 512])
    (oqm, okm, ovm, ozm, oqn, okc, ovc, oks, ovs, okw, ovw, ogl, ozn) = offs[:13]
    perm = []
    for h in range(8):
        for o in (oqm, okm, ovm, ozm):
            perm += list(range(o + 64 * h, o + 64 * h + 64))
    for g in range(2):
        perm += list(range(oqn + 256 * g, oqn + 256 * g + 256))
        for o in (okc, ovc, oks, okw, ovs, ovw):
            perm += list(range(o + 64 * g, o + 64 * g + 64))
        perm += list(range(ogl + 12 * g, ogl + 12 * g + 12))
        perm += list(range(ozn + 256 * g, ozn + 256 * g + 256))
    assert len(perm) == DIN and len(set(perm)) == DIN
    return np.array(perm)


STAGE = 99
ATTACH_WAITS = True
TVAR = 2
SUB = 99


def build(NB, L, consts, debug=False):
    nc = bass.Bass("TRN2", target_bir_lowering=False)

    def din(name, shape, dt=F32):
        return nc.dram_tensor(name, list(shape), dt, kind="ExternalInput").ap()

    x_d = din("x", [NB, S, D])
    cT_d = din("cT", [128, 8, NB])
    wada_d = din("w_ada", [L, D, 3 * D])
    badaT_d = din("b_adaT", [128, L, 24])
    gpreT_d = din("g_preT", [128, L, 8])
    gpostT_d = din("g_postT", [128, L, 8])
    win_d = din("w_in", [L, D, DIN])
    wout_d = din("w_out", [L, D, D])
    wc1_d = {"k": din("wc1k", [L, 64, 2048]), "v": din("wc1v", [L, 64, 2048])}
    peT_d = {"k": din("peTk", [L, 64, 32]), "v": din("peTv", [L, 64, 32])}
    w2_d = {"k": din("w2k", [L, 64, 64]), "v": din("w2v", [L, 64, 64])}
    cd = {}
    for k, v in consts.items():
        cd[k] = din("c_" + k, v.shape, BF16 if v.dtype == ml_dtypes.bfloat16 else F32)
    y_d = nc.dram_tensor("y", [NB, S, D], F32, kind="ExternalOutput").ap()
    if debug:
        dbg_d = nc.dram_tensor("dbg", [S, D], BF16, kind="ExternalOutput").ap()

    with contextlib.ExitStack() as es:
        es.enter_context(nc.allow_low_precision("bf16 matmul operands, fp32 accumulation"))
        sy = Sync(nc, es)
        op, dma = sy.op, sy.dma

        def sb(name, shape, dt):
            return es.enter_context(nc.sbuf_tensor(name, list(shape), dt))

        def ps(name, shape, dt):
            return es.enter_context(nc.psum_tensor(name, list(shape), dt))

        HT = sb("HT", [128, 8, S], BF16);            bHT = [Buf("HT%d" % i) for i in range(4)]
        Y = sb("Y", [128, NT, D], BF16);             bY = [Buf("Y%d" % i) for i in range(NT)]
        ident = sb("ident", [128, 128], BF16);       bC = Buf("consts")
        identf = sb("identf", [128, 128], F32)
        onesf = sb("onesf", [128, 128], F32)
        TRI = sb("TRI", [128, 512], BF16)
        TRIC4 = sb("TRIC4", [128, 512], BF16)
        TRIA4 = sb("TRIA4", [128, 512], BF16)
        SH = sb("SH", [128, 256], BF16)
        TC = sb("TC", [128, 512], BF16)
        CTAB = sb("CTAB", [128, NT, 32], F32)
        CPAD = sb("CPAD", [128, NT, 8], F32)
        NEGH = sb("NEGH", [128, 16], F32)
        WST = [sb("WST%d" % i, [128, 8, 256], F32) for i in range(1)]
        bWST = [Buf("WST0")]
        wst_i = [0]
        WBm = sb("WBm", [128, 8, 256], BF16);        bWBm = Buf("WBm")
        WBn = sb("WBn", [128, 8, 512], BF16);        bWBn = Buf("WBn")
        WC1 = {k: sb("WC1" + k, [64, 32, 64], BF16) for k in "kv"}
        PEb = {k: sb("PE" + k, [64, 32], BF16) for k in "kv"}
        W2 = {k: sb("W2" + k, [64, 64], BF16) for k in "kv"}
        bWC = Buf("WC")
        QA = sb("QA", [128, S], BF16);               bQA = [Buf("QA%d" % i) for i in range(4)]
        KA = sb("KA", [128, S], BF16);               bKA = Buf("KA")
        VA = sb("VA", [128, NT, 65], BF16);          bVA = Buf("VA")
        SZ = sb("SZ", [128, NT, 64], BF16);          bSZ = Buf("SZ")
        KMf = sb("KMf", [64, 8], F32);               bKM = Buf("KM")
        KMb = sb("KMb", [64, 8], BF16)
        GP = sb("GP", [128, NT, 8], F32);            bGP = Buf("GP")
        M8 = sb("M8", [128, 8], F32);                bM8 = Buf("M8")
        M8b = sb("M8b", [128, 8], F32);              bM8b = Buf("M8b")
        MB = sb("MB", [128, NT, 96], BF16);          bMB = [Buf("MB%d" % i) for i in range(4)]
        QAn = sb("QAn", [128, NT * 512], BF16);      bQAn = Buf("QAn")
        KAs = sb("KAs", [128, S], BF16);             bKAs = Buf("KAs")
        KAw = sb("KAw", [128, S], BF16);             bKAw = Buf("KAw")
        VSW = sb("VSW", [128, NT, 2, 65], BF16);     bVSW = Buf("VSW")
        KCA = sb("KCA", [128, 128], BF16);           bKCA = Buf("KCA")
        VCA = sb("VCA", [128, 97], BF16);            bVCA = Buf("VCA")
        KCR1 = sb("KCR", [64, S], BF16)
        KCR = {k: KCR1 for k in "kv"}
        bKCR1 = Buf("KCR")
        bKCR = {k: bKCR1 for k in "kv"}
        SZn = sb("SZn", [128, NT, 256], BF16);       bSZn = Buf("SZn")
        GT = sb("GT", [128, NT, 12], F32);           bGT = Buf("GT")
        OC = sb("OC", [128, NT, 256], BF16);          bOC = [Buf("OC%d" % i) for i in range(NT)]
        IMP = sb("IMP", [128, NT, 32], F32);         bIMP = [Buf("IMP%d" % i) for i in range(NT)]
        TMP32 = sb("TMP32", [128, 32], F32);         bTMP32 = Buf("TMP32")
        XG = sb("XG", [64, 128], F32);               bXG = Buf("XG")
        XG2 = sb("XG2", [64, 128], F32);             bXG2 = Buf("XG2")
        GK = sb("GK", [64, 128], BF16);              bGK = Buf("GK")
        CB = sb("CB", [64, 2], F32);                 bCB = Buf("CB")
        PT = [sb("PT%d" % i, [128, 512], BF16) for i in range(5)]
        bPT = [Buf("PT%d" % i) for i in range(5)]
        pt_i = [0]
        XT = [sb("XT%d" % i, [128, D], F32) for i in range(2)]
        bXT = [Buf("XT0"), Buf("XT1")]
        xt_i = [0]
        XN = sb("XN", [128, D], BF16);               bXN = Buf("XN")
        JUNK = sb("JUNK", [128, D], BF16);           bJUNK = Buf("JUNK")
        YT = sb("YT", [128, 8, 128], BF16);          bYT = Buf("YT")
        OT = sb("OT", [128, D], F32);                bOT = Buf("OT")
        GG = sb("GG", [128, D], F32);                bGG = Buf("GG")
        DG = sb("DG", [128, 128], F32);              bDG = Buf("DG")
        TZ = sb("TZ", [128, 256], F32);              bTZ = Buf("TZ")
        SS = sb("SS", [128, NT], F32);               bSS = Buf("SS")
        RSTD = sb("RSTD", [128, NT], F32);           bRSTD = Buf("RSTD")
        SS2 = sb("SS2", [128, 4], F32);              bSS2 = Buf("SS2")
        RS = sb("RS", [128, 8, 1], F32);             bRS = Buf("RS")
        AC = sb("AC", [128, 4, 1], F32);             bAC = Buf("AC")
        TMPF = sb("TMPF", [128, 256], F32);          bTMPF = Buf("TMPF")
        CST = sb("CST", [128, 8, NB], F32);          bCST = Buf("CST")
        CSA = sb("CSA", [128, 8, NB], F32)
        MODT = sb("MODT", [128, L, 24, NB], F32);    bMOD = Buf("MOD")
        BADA = sb("BADA", [128, L, 24], F32)
        GPRE = sb("GPRE", [128, L, 8], F32)
        GPOST = sb("GPOST", [128, L, 8], F32)
        AMOD = sb("AMOD", [128, L, NB, 8], F32)
        GGT = sb("GGT", [128, L, NB, 8], F32)
        SC = [ps("SC%d" % i, [128, 512], F32) for i in range(3)]
        bSC = [Buf("SC%d" % i, True) for i in range(3)]
        sc_i = [0]
        OAB = [ps("OA%d" % i, [128, 512], F32) for i in range(2)]
        bOAB = [Buf("OA0", True), Buf("OA1", True)]
        oa_i = [0]
        PJ = [ps("PJ%d" % i, [128, 512], F32) for i in range(2)]
        bPJ = [Buf("PJ0", True), Buf("PJ1", True)]
        pj_i = [0]
        TP = ps("TP", [128, 1024], BF16);            bTP = Buf("TP", True)

        bYD = [[Buf("YD") for _ in range(NT)] for _ in range(NB)]

        def nxt(lst, bl, ctr):
            i = ctr[0] % len(lst)
            ctr[0] += 1
            return lst[i], bl[i]

        for t_, k in ((ident, "ident"), (identf, "identf"), (onesf, "onesf"), (TRI, "tri"), (TRIC4, "tric4"),
                      (TRIA4, "tria4"), (SH, "sh"), (TC, "tc"), (CTAB, "ctab"), (CPAD, "cpad")):
            dma(t_[:], cd[k], writes=[bC])
        op("pool", lambda: nc.gpsimd.memset(NEGH[:], -0.5), writes=[bC])
        dma(BADA[:], badaT_d, writes=[bC])
        dma(GPRE[:], gpreT_d, writes=[bC])
        dma(GPOST[:], gpostT_d, writes=[bC])
        for t_, b_ in ((QA, bQA), (KA, [bKA]), (KAs, [bKAs]), (KAw, [bKAw]), (KCA, [bKCA]), (QAn, [bQAn])):
            op("pool", lambda t_=t_: nc.gpsimd.memset(t_[:], 0.0), writes=b_)
        op("pool", lambda: nc.gpsimd.memset(MB[:], 0.0), writes=bMB)
        op("pool", lambda: nc.gpsimd.memset(VA[:], 2.0), writes=[bVA])
        op("pool", lambda: nc.gpsimd.memset(VSW[:], 1.0), writes=[bVSW])
        op("pool", lambda: nc.gpsimd.memset(VCA[:], 0.0), writes=[bVCA])
        dma(KA[64:72, :], cd["ind8"], writes=[bKA])
        dma(KA[96:100, :], cd["kcon"], writes=[bKA])
        dma(KAs[64:96, :], cd["ind32"], writes=[bKAs])
        dma(KAs[96:100, :], cd["kcon"], writes=[bKAs])
        dma(KAw[96:100, :], cd["kcon"], writes=[bKAw])
        dma(KCA[96:100, 0:127], cd["kccon"], writes=[bKCA])
        dma(VCA[0:127, 64:97], cd["vcc"], writes=[bVCA])

        def load_cast(dst_ap_fn, src_ap, ncols, bdst, nrow_p=128):
            wst, bw = nxt(WST, bWST, wst_i)
            dma(wst[0:nrow_p, :, 0:ncols], src_ap, writes=[bw])
            op("pool", lambda: nc.gpsimd.tensor_copy(out=dst_ap_fn(), in_=wst[0:nrow_p, :, 0:ncols]),
               reads=[bw], writes=[bdst])

        def silu_parts(out_ap, z_ps_ap, bps, bout, tz_ap):
            op("act", lambda: nc.scalar.activation(out=tz_ap, in_=z_ps_ap, func=AF.Tanh, scale=0.5),
               reads=[bps], writes=[bTZ])
            op("dve", lambda: nc.vector.scalar_tensor_tensor(out=out_ap, in0=tz_ap, scalar=1.0, in1=z_ps_ap,
                                                              op0=ALU.add, op1=ALU.mult),
               reads=[bTZ, bps], writes=[bout])

        def mm(out, lhsT, rhs, start, stop, reads, writes, skip=False):
            op("pe", lambda: nc.tensor.matmul(out, lhsT, rhs, start=start, stop=stop, skip_group_check=skip),
               reads=reads, writes=writes)

        dma(CST[:], cT_d, writes=[bCST])
        op("act", lambda: nc.scalar.activation(out=CSA[:], in_=CST[:], func=AF.Tanh, scale=0.5),
           reads=[bCST], writes=[bMOD])
        op("dve", lambda: nc.vector.scalar_tensor_tensor(out=CSA[:], in0=CSA[:], scalar=1.0, in1=CST[:],
                                                          op0=ALU.add, op1=ALU.mult), reads=[bMOD, bCST], writes=[bMOD])
        op("dve", lambda: nc.vector.tensor_scalar(out=CSA[:], in0=CSA[:], scalar1=0.5, scalar2=None, op0=ALU.mult),
           reads=[bMOD], writes=[bMOD])
        bCSA = bMOD
        for l in range(L):
            for blk in range(12):
                wst, bw = nxt(WST, bWST, wst_i)
                dma(wst[:, :, :], wada_d[l, :, blk * 256:(blk + 1) * 256].rearrange("(kc p) c -> p kc c", p=128),
                    writes=[bw])
                pj, bpj = nxt(PJ, bPJ, pj_i)
                for half in range(2):
                    for kc in range(8):
                        mm(pj[:, half * NB:(half + 1) * NB], wst[:, kc, half * 128:(half + 1) * 128], CSA[:, kc, :],
                           start=(kc == 0), stop=(kc == 7), reads=[bw, bCSA], writes=[bpj],
                           skip=True)
                for half in range(2):
                    j = 2 * blk + half
                    op("dve", lambda half=half, j=j, pj=pj: nc.vector.tensor_scalar(
                        out=MODT[:, l, j, :], in0=pj[:, half * NB:(half + 1) * NB], scalar1=BADA[:, l, j:j + 1],
                        scalar2=None, op0=ALU.add), reads=[bpj, bC], writes=[bMOD])
            for b in range(NB):
                op("dve", lambda b=b, l=l: nc.vector.scalar_tensor_tensor(
                    out=AMOD[:, l, b, :], in0=MODT[:, l, 8:16, b], scalar=1.0, in1=GPRE[:, l, :],
                    op0=ALU.add, op1=ALU.mult), reads=[bMOD, bC], writes=[bMOD])
                op("dve", lambda b=b, l=l: nc.vector.tensor_tensor(
                    out=GGT[:, l, b, :], in0=MODT[:, l, 16:24, b], in1=GPOST[:, l, :], op=ALU.mult),
                    reads=[bMOD, bC], writes=[bMOD])

        def prenorm(b, l, src_d):
            for tt in range(NT):
                xt, bx = nxt(XT, bXT, xt_i)
                dma(xt[:], src_d[b, tt * 128:(tt + 1) * 128, :], reads=[bYD[b][tt]], writes=[bx])
                op("act", lambda xt=xt, tt=tt: nc.scalar.activation(out=JUNK[:], in_=xt[:], func=AF.Square,
                                                                     accum_out=SS[:, tt:tt + 1]),
                   reads=[bx], writes=[bJUNK, bSS], attach=False)
                op("dve", lambda tt=tt: nc.vector.tensor_scalar(out=SS[:, tt:tt + 1], in0=SS[:, tt:tt + 1],
                                                                 scalar1=1.0 / D, scalar2=EPS, op0=ALU.mult, op1=ALU.add),
                   reads=[bSS], writes=[bSS])
                op("pool", lambda tt=tt: nc.gpsimd.tensor_tensor(out=RSTD[:, tt:tt + 1], in0=SS[:, tt:tt + 1],
                                                                  in1=NEGH[:, 0:1], op=ALU.pow),
                   reads=[bSS, bC], writes=[bRSTD])
                op("act", lambda xt=xt, tt=tt: nc.scalar.activation(out=XN[:], in_=xt[:], func=AF.Identity,
                                                                     scale=RSTD[:, tt:tt + 1]),
                   reads=[bx, bRSTD], writes=[bXN])
                for kc in range(8):
                    op("pe", lambda kc=kc: nc.tensor.transpose(out=TP[:, kc * 128:(kc + 1) * 128],
                                                               in_=XN[:, kc * 128:(kc + 1) * 128], identity=ident[:]),
                       reads=[bXN, bC], writes=[bTP])
                for kc in range(8):
                    e = "dve" if kc % 2 == 0 else "act"
                    if e == "dve":
                        op("dve", lambda kc=kc, tt=tt: nc.vector.tensor_scalar(
                            out=HT[:, kc, tt * 128:(tt + 1) * 128], in0=TP[:, kc * 128:(kc + 1) * 128],
                            scalar1=AMOD[:, l, b, kc:kc + 1], scalar2=MODT[:, l, kc, b:b + 1],
                            op0=ALU.mult, op1=ALU.add), reads=[bTP, bMOD], writes=[bHT[tt // 4]])
                    else:
                        op("act", lambda kc=kc, tt=tt: nc.scalar.activation(
                            out=HT[:, kc, tt * 128:(tt + 1) * 128], in_=TP[:, kc * 128:(kc + 1) * 128],
                            func=AF.Identity, scale=AMOD[:, l, b, kc:kc + 1], bias=MODT[:, l, kc, b:b + 1]),
                            reads=[bTP, bMOD], writes=[bHT[tt // 4]])

        def proj_fm(dst_fn, wb, c0, scale, bw, bdst_fn, sview=None):
            for c in range(4):
                pj, bpj = nxt(PJ, bPJ, pj_i)
                for kc in range(8):
                    mm(pj[0:64, :], wb[:, kc, c0:c0 + 64], HT[:, kc, c * 512:(c + 1) * 512],
                       start=(kc == 0), stop=(kc == 7), reads=[bw, bHT[c]], writes=[bpj])
                e = "act" if c % 2 == 0 else "dve"
                src = pj[0:64, :] if sview is None else sview(pj[0:64, :])
                if e == "act":
                    op("act", lambda c=c, src=src: nc.scalar.mul(out=dst_fn(c), in_=src, mul=scale),
                       reads=[bpj], writes=bdst_fn(c))
                else:
                    op("dve", lambda c=c, src=src: nc.vector.tensor_scalar(out=dst_fn(c), in0=src, scalar1=scale,
                                                                            scalar2=None, op0=ALU.mult),
                       reads=[bpj], writes=bdst_fn(c))

        def exp_tile(sc, bsc, rows, c0, c1):
            pt, bpt = nxt(PT, bPT, pt_i)
            op("act", lambda: nc.scalar.activation(out=pt[0:rows, c0:c1], in_=sc[0:rows, c0:c1], func=AF.Exp),
               reads=[bsc], writes=[bpt])
            return pt, bpt

        SCX = SC + PJ
        bSCX = bSC + bPJ

        def run_tiles(tiles, look=4):
            q = []

            def emit_s(t):
                sc, bsc = nxt(SCX, bSCX, sc_i)
                n = len(t["s"])
                for k, (lhsT, rhs, rds) in enumerate(t["s"]):
                    mm(sc[0:t["rows"], t["c0"]:t["c1"]], lhsT, rhs, start=(k == 0), stop=(k == n - 1), reads=rds,
                       writes=[bsc])
                return sc, bsc

            def emit_rest(t, sc, bsc):
                pt, bpt = exp_tile(sc, bsc, t["rows"], t["c0"], t["c1"])
                for (out, lo, hi, rhs, st, sp, rds, boa) in t["pv"]:
                    mm(out, pt[0:t["rows"], lo:hi], rhs, start=st, stop=sp, reads=[bpt] + rds, writes=[boa], skip=True)
                if t.get("after") is not None:
                    t["after"]()

            for t in tiles:
                sc, bsc = emit_s(t)
                q.append((t, sc, bsc))
                if len(q) > look:
                    emit_rest(*q.pop(0))
            while q:
                emit_rest(*q.pop(0))

        def moba_head(b, l, h):
            col0 = 256 * h
            load_cast(lambda: WBm[:, :, :], win_d[l, :, col0:col0 + 256].rearrange("(kc p) c -> p kc c", p=128),
                      256, bWBm)
            dma(QA[96:100, :], cd["qcm"][h], writes=bQA)
            proj_fm(lambda c: QA[0:64, c * 512:(c + 1) * 512], WBm, 0, 0.125, bWBm, lambda c: [bQA[c]])
            proj_fm(lambda c: KA[0:64, c * 512:(c + 1) * 512], WBm, 64, 1.0, bWBm, lambda c: [bKA])
            for g4 in range(4):
                pj, bpj = nxt(PJ, bPJ, pj_i)
                for t4 in range(4):
                    tt = g4 * 4 + t4
                    for kc in range(8):
                        mm(pj[:, t4 * 128:(t4 + 1) * 128], HT[:, kc, tt * 128:(tt + 1) * 128], WBm[:, kc, 128:256],
                           start=(kc == 0), stop=(kc == 7), reads=[bHT[g4], bWBm], writes=[bpj], skip=True)
                pjv = pj[:, :].rearrange("p (t c) -> p t c", c=128)
                op("dve", lambda pjv=pjv, g4=g4: nc.vector.tensor_copy(out=VA[:, g4 * 4:(g4 + 1) * 4, 0:64],
                                                                       in_=pjv[:, :, 0:64]),
                   reads=[bpj], writes=[bVA])
                tzv = TZ[:, 0:256].rearrange("p (t c) -> p t c", c=64)
                silu_parts(SZ[:, g4 * 4:(g4 + 1) * 4, :], pjv[:, :, 64:128], bpj, bSZ, tzv)
            op("dve", lambda: nc.vector.tensor_reduce(out=KMf[:, :], in_=KA[0:64, :].rearrange("p (n k) -> p n k", k=256),
                                                      axis=AX.X, op=ALU.add), reads=[bKA], writes=[bKM])
            op("dve", lambda: nc.vector.tensor_scalar(out=KMb[:, :], in0=KMf[:, :], scalar1=1.0 / 256, scalar2=None,
                                                      op0=ALU.mult), reads=[bKM], writes=[bKM])
            pj, bpj = nxt(PJ, bPJ, pj_i)
            for tt in range(NT):
                mm(pj[:, tt * 8:(tt + 1) * 8], QA[0:64, tt * 128:(tt + 1) * 128], KMb[:, :], start=True, stop=True,
                   reads=[bQA[tt // 4], bKM], writes=[bpj], skip=True)
            op("dve", lambda pj=pj: nc.vector.tensor_tensor(out=GP[:], in0=pj[:, 0:128].rearrange("p (t n) -> p t n", n=8),
                                                            in1=CPAD[:], op=ALU.add), reads=[bpj, bC], writes=[bGP])
            op("dve", lambda: nc.vector.tensor_scalar(out=MB[:, 0:6, 64:72], in0=GP[:, 0:6, :], scalar1=-1e8, scalar2=NEGM,
                                                      op0=ALU.is_lt, op1=ALU.mult), reads=[bGP], writes=[bMB[0], bMB[1]])
            for tt in range(6, NT):
                op("dve", lambda tt=tt: nc.vector.max(out=M8[:, :], in_=GP[:, tt, :]), reads=[bGP], writes=[bM8])
                op("dve", lambda tt=tt: nc.vector.tensor_scalar(out=MB[:, tt, 64:72], in0=GP[:, tt, :], scalar1=M8[:, 3:4],
                                                                 scalar2=NEGM, op0=ALU.is_lt, op1=ALU.mult),
                   reads=[bGP, bM8], writes=[bMB[tt // 4]])
            for c in range(4):
                for t4 in range(4):
                    tt = 4 * c + t4
                    op("pe", lambda tt=tt, t4=t4: nc.tensor.transpose(out=TP[0:96, t4 * 128:(t4 + 1) * 128],
                                                                      in_=MB[:, tt, :], identity=ident[:]),
                       reads=[bMB[c], bC], writes=[bTP])
                op("act", lambda c=c: nc.scalar.copy(out=QA[64:72, c * 512:(c + 1) * 512], in_=TP[64:72, 0:512]),
                   reads=[bTP], writes=[bQA[c]])
            tiles = []
            for c in range(4):
                oa, boa = nxt(OAB, bOAB, oa_i)
                oav = oa[:, 0:260].rearrange("p (j c) -> p j c", c=65)
                nkt = 4 * c + 4

                def epi(c=c, oav=oav, boa=boa):
                    op("dve", lambda: nc.vector.reciprocal(out=RS[:, 0:4, :], in_=oav[:, :, 64:65]),
                       reads=[boa], writes=[bRS])
                    for j2 in range(4):
                        tt = 4 * c + j2
                        op("dve", lambda j2=j2, tt=tt: nc.vector.scalar_tensor_tensor(
                            out=Y[:, tt, h * 64:(h + 1) * 64], in0=oav[:, j2, 0:64], scalar=RS[:, j2, :],
                            in1=SZ[:, tt, :], op0=ALU.mult, op1=ALU.mult), reads=[boa, bRS, bSZ], writes=[bY[tt]])

                for kt in range(nkt):
                    j0 = max(0, kt - 4 * c)
                    c0 = 128 * j0
                    diag = kt >= 4 * c
                    smm = [(KA[0:100, kt * 128:(kt + 1) * 128], QA[0:100, c * 512 + c0:(c + 1) * 512], [bKA, bQA[c]])]
                    if diag:
                        smm.append((ident[:, :], TRI[:, 0:512 - c0], [bC]))
                    pv = []
                    for j2 in range(j0, 4):
                        pv.append((oav[:, j2, :], j2 * 128, (j2 + 1) * 128, VA[:, kt, :], (kt == 0 and j2 == 0),
                                   (kt == 4 * c + j2), [bVA], boa))
                    tiles.append(dict(rows=128, c0=c0, c1=512, s=smm, pv=pv, after=epi if kt == nkt - 1 else None))
            run_tiles(tiles)

        def gelu_fm(src_ps, bsrc, bias_ap, out_bf):
            op("act", lambda: nc.scalar.activation(out=XG[:, 0:127], in_=src_ps, func=AF.Identity, bias=bias_ap),
               reads=[bsrc, bCB], writes=[bXG])
            op("dve", lambda: nc.vector.tensor_tensor(out=XG2[:, 0:127], in0=XG[:, 0:127], in1=XG[:, 0:127], op=ALU.mult),
               reads=[bXG], writes=[bXG2])
            op("dve", lambda: nc.vector.tensor_scalar(out=XG2[:, 0:127], in0=XG2[:, 0:127], scalar1=0.044715, scalar2=1.0,
                                                      op0=ALU.mult, op1=ALU.add), reads=[bXG2], writes=[bXG2])
            op("dve", lambda: nc.vector.tensor_tensor(out=XG2[:, 0:127], in0=XG2[:, 0:127], in1=XG[:, 0:127], op=ALU.mult),
               reads=[bXG2, bXG], writes=[bXG2])
            op("act", lambda: nc.scalar.activation(out=XG2[:, 0:127], in_=XG2[:, 0:127], func=AF.Tanh,
                                                   scale=0.7978845608028654), reads=[bXG2], writes=[bXG2])
            op("dve", lambda: nc.vector.scalar_tensor_tensor(out=XG2[:, 0:127], in0=XG2[:, 0:127], scalar=1.0,
                                                              in1=XG[:, 0:127], op0=ALU.add, op1=ALU.mult),
               reads=[bXG2, bXG], writes=[bXG2])
            op("dve", lambda: nc.vector.tensor_scalar(out=out_bf, in0=XG2[:, 0:127], scalar1=0.5, scalar2=None,
                                                      op0=ALU.mult), reads=[bXG2], writes=[bGK])

        def nsa_group(b, l, g):
            col0 = 2048 + 908 * g
            for (a0, a1) in ((0, 256), (256, 512)):
                load_cast(lambda a0=a0, a1=a1: WBn[:, :, a0:a1],
                          win_d[l, :, col0 + a0:col0 + a1].rearrange("(kc p) c -> p kc c", p=128), a1 - a0, bWBn)
            QAv = QAn[:, :].rearrange("p (t j i) -> p t j i", j=4, i=128)

            def compress():
                for kv, wc0 in (("k", 256), ("v", 320)):
                    proj_fm(lambda c, kv=kv: KCR[kv][:, c * 512:(c + 1) * 512], WBn, wc0, 1.0, bWBn,
                            lambda c, kv=kv: [bKCR[kv]])
                    kcr = KCR[kv][:, :].rearrange("p (t r) -> p t r", r=16)
                    pj, bpj = nxt(PJ, bPJ, pj_i)
                    for l_ in range(32):
                        mm(pj[0:64, 200:201], WC1[kv][:, l_, :], PEb[kv][:, l_:l_ + 1], start=(l_ == 0), stop=(l_ == 31),
                           reads=[bWC], writes=[bpj], skip=True)
                    ci = 0 if kv == "k" else 1
                    op("dve", lambda pj=pj, ci=ci: nc.vector.tensor_copy(out=CB[:, ci:ci + 1], in_=pj[0:64, 200:201]),
                       reads=[bpj], writes=[bCB])
                    pj2, bpj2 = nxt(PJ, bPJ, pj_i)
                    for l_ in range(32):
                        rhs = kcr[:, 0:127, l_] if l_ < 16 else kcr[:, 1:128, l_ - 16]
                        mm(pj2[0:64, 0:127], WC1[kv][:, l_, :], rhs, start=(l_ == 0), stop=(l_ == 31),
                           reads=[bWC, bKCR[kv]], writes=[bpj2])
                    gelu_fm(pj2[0:64, 0:127], bpj2, CB[:, ci:ci + 1], GK[:, 0:127])
                    pj3, bpj3 = nxt(PJ, bPJ, pj_i)
                    if kv == "k":
                        mm(pj3[0:64, 0:127], W2["k"][:, :], GK[:, 0:127], start=True, stop=True, reads=[bWC, bGK],
                           writes=[bpj3])
                        op("dve", lambda pj3=pj3: nc.vector.tensor_copy(out=KCA[0:64, 0:127], in_=pj3[0:64, 0:127]),
                           reads=[bpj3], writes=[bKCA])
                    else:
                        mm(pj3[0:127, 0:64], GK[:, 0:127], W2["v"][:, :], start=True, stop=True, reads=[bWC, bGK],
                           writes=[bpj3])
                        op("dve", lambda pj3=pj3: nc.vector.tensor_copy(out=VCA[0:127, 0:64], in_=pj3[0:127, 0:64]),
                           reads=[bpj3], writes=[bVCA])


            dma(QAn[96:100, :], cd["qcn"][g], writes=[bQAn])
            for j in range(4):
                proj_fm(lambda c, j=j: QAv[0:64, 4 * c:4 * c + 4, j, :], WBn, 64 * j, 0.125, bWBn, lambda c: [bQAn],
                        sview=lambda a: a.rearrange("p (t i) -> p t i", i=128))
            proj_fm(lambda c: KAs[0:64, c * 512:(c + 1) * 512], WBn, 384, 1.0, bWBn, lambda c: [bKAs])
            proj_fm(lambda c: KAw[0:64, c * 512:(c + 1) * 512], WBn, 448, 1.0, bWBn, lambda c: [bKAw])
            if SUB < 1:
                return
            compress()
            if SUB < 2:
                return
            for (a0, a1) in ((512, 768), (768, 908)):
                load_cast(lambda a0=a0, a1=a1: WBn[:, :, a0 - 512:a1 - 512],
                          win_d[l, :, col0 + a0:col0 + a1].rearrange("(kc p) c -> p kc c", p=128), a1 - a0, bWBn)
            for tt in range(NT):
                pj, bpj = nxt(PJ, bPJ, pj_i)
                for kc in range(8):
                    mm(pj[:, 0:396], HT[:, kc, tt * 128:(tt + 1) * 128], WBn[:, kc, 0:396],
                       start=(kc == 0), stop=(kc == 7), reads=[bHT[tt // 4], bWBn], writes=[bpj])
                op("dve", lambda pj=pj, tt=tt: nc.vector.tensor_copy(
                    out=VSW[:, tt, :, 0:64], in_=pj[:, 0:128].rearrange("p (a c) -> p a c", c=64)),
                    reads=[bpj], writes=[bVSW])
                op("act", lambda pj=pj, tt=tt: nc.scalar.activation(out=GT[:, tt, :], in_=pj[:, 128:140], func=AF.Tanh,
                                                                     scale=0.5), reads=[bpj], writes=[bGT])
                silu_parts(SZn[:, tt, :], pj[:, 140:396], bpj, bSZn, TZ[:, 0:256])
            op("dve", lambda: nc.vector.tensor_scalar(out=GT[:], in0=GT[:], scalar1=0.5, scalar2=0.5, op0=ALU.mult,
                                                      op1=ALU.add), reads=[bGT], writes=[bGT])
            def branch_epilogue(oav, boa, tt, gi, first, last):
                op("dve", lambda: nc.vector.tensor_scalar(out=RS[:, 0:4, :], in0=oav[:, :, 64:65], scalar1=1e-30,
                                                          scalar2=None, op0=ALU.add), reads=[boa], writes=[bRS])
                op("dve", lambda: nc.vector.reciprocal(out=RS[:, 0:4, :], in_=RS[:, 0:4, :]), reads=[bRS], writes=[bRS])
                gv = GT[:, tt, :].rearrange("p (j r) -> p j r", r=3)
                op("dve", lambda: nc.vector.tensor_tensor(out=AC[:, :, :], in0=RS[:, 0:4, :], in1=gv[:, :, gi:gi + 1],
                                                          op=ALU.mult), reads=[bRS, bGT], writes=[bAC])
                ocv = OC[:, tt, :].rearrange("p (j c) -> p j c", c=64)
                for j in range(4):
                    if first:
                        op("dve", lambda j=j: nc.vector.tensor_scalar(out=ocv[:, j, :], in0=oav[:, j, 0:64],
                                                                      scalar1=AC[:, j, :], scalar2=None, op0=ALU.mult),
                           reads=[boa, bAC], writes=[bOC[tt]])
                    else:
                        op("dve", lambda j=j: nc.vector.scalar_tensor_tensor(
                            out=ocv[:, j, :], in0=oav[:, j, 0:64], scalar=AC[:, j, :], in1=ocv[:, j, :],
                            op0=ALU.mult, op1=ALU.add), reads=[boa, bAC, bOC[tt]], writes=[bOC[tt]])
                if last:
                    yc = 512 + 256 * g
                    op("dve", lambda: nc.vector.scalar_tensor_tensor(
                        out=Y[:, tt, yc:yc + 256], in0=OC[:, tt, :], scalar=0.5, in1=SZn[:, tt, :],
                        op0=ALU.mult, op1=ALU.mult), reads=[bOC[tt], bSZn], writes=[bY[tt]])

            if SUB < 3:
                return
            tiles = []
            for tt in range(NT):
                oa, boa = nxt(OAB, bOAB, oa_i)
                oav = oa[:, 0:388].rearrange("p (j c) -> p j c", c=97)

                def epiA(tt=tt, oav=oav, boa=boa):
                    branch_epilogue(oav, boa, tt, 0, True, False)
                    for j in range(4):
                        in1 = CTAB[:, tt, :] if j == 0 else IMP[:, tt, :]
                        op("dve", lambda j=j, in1=in1: nc.vector.scalar_tensor_tensor(
                            out=IMP[:, tt, :], in0=oav[:, j, 65:97], scalar=RS[:, j, :], in1=in1,
                            op0=ALU.mult, op1=ALU.add), reads=[boa, bRS, bC, bIMP[tt]], writes=[bIMP[tt]])
                    if tt >= 8:
                        op("dve", lambda: nc.vector.max(out=M8[:, :], in_=IMP[:, tt, :]), reads=[bIMP[tt]], writes=[bM8])
                        op("dve", lambda: nc.vector.match_replace(out=TMP32[:, :], in_to_replace=M8[:, :],
                                                                  in_values=IMP[:, tt, :], imm_value=-3e38),
                           reads=[bM8, bIMP[tt]], writes=[bTMP32], attach=False)
                        op("dve", lambda: nc.vector.max(out=M8b[:, :], in_=TMP32[:, :]), reads=[bTMP32], writes=[bM8b])
                        op("dve", lambda: nc.vector.tensor_scalar(out=MB[:, tt, 64:96], in0=IMP[:, tt, :],
                                                                  scalar1=M8b[:, 7:8], scalar2=NEGM, op0=ALU.is_lt,
                                                                  op1=ALU.mult), reads=[bIMP[tt], bM8b],
                           writes=[bMB[tt // 4]])
                    else:
                        op("dve", lambda: nc.vector.tensor_scalar(out=MB[:, tt, 64:96], in0=IMP[:, tt, :], scalar1=-1e8,
                                                                  scalar2=NEGM, op0=ALU.is_lt, op1=ALU.mult),
                           reads=[bIMP[tt]], writes=[bMB[tt // 4]])

                smm = [(KCA[0:100, 0:127], QAn[0:100, tt * 512:(tt + 1) * 512], [bKCA, bQAn]),
                       (SH[:, 128 - 8 * tt:128 - 8 * tt + 127], TC[:, :], [bC])]
                pv = [(oav[:, j, :], j * 128, (j + 1) * 128, VCA[0:127, :], (j == 0), True, [bVCA], boa) for j in range(4)]
                tiles.append(dict(rows=127, c0=0, c1=512, s=smm, pv=pv, after=epiA))
            if SUB < 4:
                run_tiles(tiles)
                return
            for tt in range(NT):
                oa, boa = nxt(OAB, bOAB, oa_i)
                oav = oa[:, 0:260].rearrange("p (j c) -> p j c", c=65)
                kts = [kt for kt in range(tt - 4, tt + 1) if kt >= 0]

                def epiW(tt=tt, oav=oav, boa=boa):
                    branch_epilogue(oav, boa, tt, 2, False, False)

                for kt in kts:
                    extra = TRIC4 if kt == tt else (TRIA4 if kt == tt - 4 else None)
                    smm = [(KAw[0:100, kt * 128:(kt + 1) * 128], QAn[0:100, tt * 512:(tt + 1) * 512], [bKAw, bQAn])]
                    if extra is not None:
                        smm.append((ident[:, :], extra[:, :], [bC]))
                    pv = [(oav[:, j, :], j * 128, (j + 1) * 128, VSW[:, kt, 1, :], (kt == kts[0] and j == 0),
                           (kt == kts[-1]), [bVSW], boa) for j in range(4)]
                    tiles.append(dict(rows=128, c0=0, c1=512, s=smm, pv=pv, after=epiW if kt == kts[-1] else None))
            run_tiles(tiles)
            if SUB < 5:
                return
            for c in range(4):
                for t4 in range(4):
                    tt = 4 * c + t4
                    op("pe", lambda tt=tt, t4=t4: nc.tensor.transpose(out=TP[0:96, t4 * 128:(t4 + 1) * 128],
                                                                      in_=MB[:, tt, :], identity=ident[:]),
                       reads=[bMB[c], bC], writes=[bTP])
                tpv = TP[64:96, 0:512].rearrange("p (t i) -> p t i", i=128)
                if TVAR == 1:
                    continue
                for j in range(4):
                    e = "act" if (j % 2 == 0 or TVAR == 2) else "dve"
                    if e == "act":
                        op("act", lambda j=j, c=c, tpv=tpv: nc.scalar.copy(out=QAv[64:96, 4 * c:4 * c + 4, j, :], in_=tpv),
                           reads=[bTP], writes=[bQAn])
                    else:
                        op("dve", lambda j=j, c=c, tpv=tpv: nc.vector.tensor_copy(out=QAv[64:96, 4 * c:4 * c + 4, j, :],
                                                                                  in_=tpv), reads=[bTP], writes=[bQAn])
            if SUB < 6:
                return
            tiles = []
            for tt in range(NT):
                oa, boa = nxt(OAB, bOAB, oa_i)
                oav = oa[:, 0:260].rearrange("p (j c) -> p j c", c=65)

                def epiS(tt=tt, oav=oav, boa=boa):
                    branch_epilogue(oav, boa, tt, 1, False, True)

                for kt in range(tt + 1):
                    smm = [(KAs[0:100, kt * 128:(kt + 1) * 128], QAn[0:100, tt * 512:(tt + 1) * 512], [bKAs, bQAn])]
                    if kt == tt:
                        smm.append((ident[:, :], TRIC4[:, :], [bC]))
                    pv = [(oav[:, j, :], j * 128, (j + 1) * 128, VSW[:, kt, 0, :], (kt == 0 and j == 0), (kt == tt),
                           [bVSW], boa) for j in range(4)]
                    tiles.append(dict(rows=128, c0=0, c1=512, s=smm, pv=pv, after=epiS if kt == tt else None))
            run_tiles(tiles)

        def outproj(b, l, src_d):
            WO = QAn[:, :].rearrange("p (kc c) -> p kc c", c=1024)
            for blk in range(4):
                load_cast(lambda blk=blk: WO[:, :, blk * 256:(blk + 1) * 256],
                          wout_d[l, :, blk * 256:(blk + 1) * 256].rearrange("(kc p) c -> p kc c", p=128), 256, bQAn)
            for kc in range(8):
                op("dve", lambda kc=kc: nc.vector.tensor_scalar(out=DG[:, :], in0=identf[:, :],
                                                                scalar1=GGT[:, l, b, kc:kc + 1], scalar2=None,
                                                                op0=ALU.mult), reads=[bC, bMOD], writes=[bDG])
                pj, bpj = nxt(PJ, bPJ, pj_i)
                mm(pj[:, 0:128], onesf[:, :], DG[:, :], start=True, stop=True, reads=[bC, bDG], writes=[bpj])
                op("act", lambda kc=kc, pj=pj: nc.scalar.copy(out=GG[:, kc * 128:(kc + 1) * 128], in_=pj[:, 0:128]),
                   reads=[bpj], writes=[bGG])
            for tt in range(NT):
                for kc in range(8):
                    op("pe", lambda kc=kc, tt=tt: nc.tensor.transpose(out=TP[:, kc * 128:(kc + 1) * 128],
                                                                      in_=Y[:, tt, kc * 128:(kc + 1) * 128],
                                                                      identity=ident[:]),
                       reads=[bY[tt], bC], writes=[bTP])
                op("act", lambda: nc.scalar.copy(out=YT[:, 0:4, :], in_=TP[:, 0:512].rearrange("p (k i) -> p k i", i=128)),
                   reads=[bTP], writes=[bYT])
                op("dve", lambda: nc.vector.tensor_copy(out=YT[:, 4:8, :],
                                                        in_=TP[:, 512:1024].rearrange("p (k i) -> p k i", i=128)),
                   reads=[bTP], writes=[bYT])
                xt, bx = nxt(XT, bXT, xt_i)
                dma(xt[:], src_d[b, tt * 128:(tt + 1) * 128, :], reads=[bYD[b][tt]], writes=[bx])
                pjs = []
                for half in range(2):
                    pj, bpj = nxt(PJ, bPJ, pj_i)
                    for kc in range(8):
                        mm(pj[:, :], YT[:, kc, :], WO[:, kc, half * 512:(half + 1) * 512], start=(kc == 0),
                           stop=(kc == 7), reads=[bYT, bQAn], writes=[bpj])
                    op("act", lambda pj=pj, half=half: nc.scalar.activation(
                        out=JUNK[:, half * 512:(half + 1) * 512], in_=pj[:, :], func=AF.Square,
                        accum_out=SS2[:, half:half + 1]), reads=[bpj], writes=[bJUNK, bSS2], attach=False)
                    pjs.append((pj, bpj))
                op("dve", lambda: nc.vector.tensor_tensor(out=SS2[:, 2:3], in0=SS2[:, 0:1], in1=SS2[:, 1:2], op=ALU.add),
                   reads=[bSS2], writes=[bSS2])
                op("dve", lambda: nc.vector.tensor_scalar(out=SS2[:, 2:3], in0=SS2[:, 2:3], scalar1=1.0 / D, scalar2=EPS,
                                                          op0=ALU.mult, op1=ALU.add), reads=[bSS2], writes=[bSS2])
                op("pool", lambda: nc.gpsimd.tensor_tensor(out=SS2[:, 3:4], in0=SS2[:, 2:3], in1=NEGH[:, 0:1], op=ALU.pow),
                   reads=[bSS2, bC], writes=[bSS2])
                for half in range(2):
                    pj, bpj = pjs[half]
                    sl = slice(half * 512, (half + 1) * 512)
                    op("dve", lambda pj=pj, sl=sl: nc.vector.scalar_tensor_tensor(
                        out=OT[:, sl], in0=pj[:, :], scalar=SS2[:, 3:4], in1=GG[:, sl], op0=ALU.mult, op1=ALU.mult),
                        reads=[bpj, bSS2, bGG], writes=[bOT])
                op("dve", lambda xt=xt: nc.vector.tensor_tensor(out=OT[:, :], in0=OT[:, :], in1=xt[:, :], op=ALU.add),
                   reads=[bOT, bx], writes=[bOT])
                dma(y_d[b, tt * 128:(tt + 1) * 128, :], OT[:, :], reads=[bOT], writes=[bYD[b][tt]])

        for b in range(NB):
            for l in range(L):
                src = x_d if l == 0 else y_d
                if STAGE < 2:
                    continue
                prenorm(b, l, src)
                if STAGE < 3:
                    continue
                for kv in "kv":
                    wst, bw = nxt(WST, bWST, wst_i)
                    wv = wst[0:64, :, :].rearrange("p a c -> p (a c)")
                    dma(wv, wc1_d[kv][l], writes=[bw])
                    op("pool", lambda kv=kv, wv=wv: nc.gpsimd.tensor_copy(
                        out=WC1[kv][:, :, :].rearrange("p a c -> p (a c)"), in_=wv), reads=[bw], writes=[bWC])
                    wst, bw = nxt(WST, bWST, wst_i)
                    wv2 = wst[0:64, 0, 0:32]
                    wv3 = wst[0:64, 1, 0:64]
                    dma(wv2, peT_d[kv][l], writes=[bw])
                    dma(wv3, w2_d[kv][l], writes=[bw])
                    op("pool", lambda kv=kv, wv2=wv2: nc.gpsimd.tensor_copy(out=PEb[kv][:, :], in_=wv2), reads=[bw],
                       writes=[bWC])
                    op("pool", lambda kv=kv, wv3=wv3: nc.gpsimd.tensor_copy(out=W2[kv][:, :], in_=wv3), reads=[bw],
                       writes=[bWC])
                for h in range(8 if STAGE >= 5 else (1 if STAGE == 4 else 0)):
                    moba_head(b, l, h)
                for g in range(2 if STAGE >= 7 else (1 if STAGE == 6 else 0)):
                    nsa_group(b, l, g)
                if STAGE < 8:
                    continue
                if debug and b == 0 and l == 0:
                    for tt in range(NT):
                        dma(dbg_d[tt * 128:(tt + 1) * 128, :], Y[:, tt, :], reads=[bY[tt]])
                outproj(b, l, src)
        sy.finish()
        build.stats = (sy.nops, sy.nwaits)
    return nc


def prep_shared(inputs, L):
    f = lambda a: np.ascontiguousarray(np.asarray(a, dtype=np.float32))
    perm = in_perm()
    sh = {}
    sh["w_ada"] = f(inputs["w_ada"][:L])
    sh["b_adaT"] = f(np.asarray(inputs["b_ada"][:L]).reshape(L, 24, 128).transpose(2, 0, 1))
    sh["g_preT"] = f(np.asarray(inputs["g_pre"][:L]).reshape(L, 8, 128).transpose(2, 0, 1))
    sh["g_postT"] = f(np.asarray(inputs["g_post"][:L]).reshape(L, 8, 128).transpose(2, 0, 1))
    sh["w_in"] = f(np.asarray(inputs["w_in"][:L])[:, :, perm])
    sh["w_out"] = f(inputs["w_out"][:L])
    for kv, n1, n2, npe in (("k", "w_ck1", "w_ck2", "pe_k"), ("v", "w_cv1", "w_cv2", "pe_v")):
        w1 = np.asarray(inputs[n1][:L]).reshape(L, 32, 64, 64).transpose(0, 2, 1, 3).reshape(L, 64, 2048)
        sh["wc1" + kv] = f(w1)
        sh["peT" + kv] = f(np.asarray(inputs[npe][:L]).transpose(0, 2, 1))
        sh["w2" + kv] = f(inputs[n2][:L])
    return sh


_CACHE = {}


def run(inputs, n_cores, NB, L, debug=False):
    consts = make_consts()
    key = (NB, L, debug)
    if key not in _CACHE:
        _CACHE[key] = build(NB, L, consts, debug=debug)
    nc = _CACHE[key]
    sh = prep_shared(inputs, L)
    for k, v in consts.items():
        sh["c_" + k] = v
    x = np.asarray(inputs["x"], dtype=np.float32)
    c = np.asarray(inputs["c"], dtype=np.float32)
    in_maps = []
    for i in range(n_cores):
        m = dict(sh)
        m["x"] = np.ascontiguousarray(x[i * NB:(i + 1) * NB])
        ci = c[i * NB:(i + 1) * NB]
        m["cT"] = np.ascontiguousarray(ci.reshape(NB, 8, 128).transpose(2, 1, 0))
        in_maps.append(m)
    res = run_bass_kernel_spmd(nc, in_maps, core_ids=list(range(n_cores)))
    out = np.concatenate([np.asarray(r["y"]) for r in res.results], axis=0)
    if debug:
        return out, np.asarray(res.results[0]["dbg"])
    return out


def kernel(x, c, w_ada, b_ada, g_pre, g_post, w_in, w_out, pe_k, pe_v, w_ck1, w_ck2, w_cv1, w_cv2):
    inputs = dict(x=x, c=c, w_ada=w_ada, b_ada=b_ada, g_pre=g_pre, g_post=g_post, w_in=w_in, w_out=w_out,
                  pe_k=pe_k, pe_v=pe_v, w_ck1=w_ck1, w_ck2=w_ck2, w_cv1=w_cv1, w_cv2=w_cv2)
    NB = x.shape[0] // N_CORES
    out = run(inputs, N_CORES, NB, 2)
    return out.astype(np.float32, copy=False)
```
